# Optimizing a Trainium2 kernel written in Bass

```python
import math
import jax, jax.numpy as jnp
from jax import lax
import numpy as np

D_MODEL = 2048
BATCH = 2
SEQ = 4096
DEPTH = 2

GRID_W = 64
CTX_LEN = 256
HEAD_DIM = 128
BRANCH_W = 512
N_BRANCH = 4
NA_HEADS = 4
NA_WIN_ROWS = 8
NA_WIN_COLS = 16
S5_GROUP = 16
S5_GROUPS = BRANCH_W // S5_GROUP
S5_STATE = 64
WG_Q_HEADS = 4
WG_KV_HEADS = 2
WG_WINDOW = 128
WG_BLOCK = 128
ML_HEADS = 4
ML_CHUNK = 128
FFN_DIM = 5632
ROPE_BASE = 10000.0
EPS = 1e-6
NEG_INF = -1e30

IN_SIZES = (
    NA_HEADS * HEAD_DIM, NA_HEADS * HEAD_DIM, NA_HEADS * HEAD_DIM,
    BRANCH_W,
    WG_Q_HEADS * HEAD_DIM, WG_KV_HEADS * HEAD_DIM, WG_KV_HEADS * HEAD_DIM,
    ML_HEADS * HEAD_DIM, ML_HEADS * HEAD_DIM, ML_HEADS * HEAD_DIM, ML_HEADS * HEAD_DIM,
    4 * ML_HEADS,
    N_BRANCH * D_MODEL,
)
IN_W = sum(IN_SIZES)
IN_SPLITS = tuple(sum(IN_SIZES[:i + 1]) for i in range(len(IN_SIZES) - 1))

kernel_name = "hybrid_flow_backbone_ctx_prefix"

F32 = jnp.float32


def rms_norm(x, g):
    x32 = x.astype(F32)
    y = x32 * lax.rsqrt(jnp.mean(x32 * x32, axis=-1, keepdims=True) + EPS)
    return (y * g.astype(F32)).astype(x.dtype)


def modulate(h, shift, scale):
    return h * (1 + scale) + shift


def split_heads(t, n_heads):
    return t.reshape(t.shape[0], t.shape[1], n_heads, HEAD_DIM)


def axial_rope_tables(n_tok):
    t = jnp.arange(n_tok)
    pos = jnp.stack([t // GRID_W, t % GRID_W], axis=-1).astype(F32)
    n_freq = HEAD_DIM // 4
    inv_freq = ROPE_BASE ** (-jnp.arange(n_freq, dtype=F32) / n_freq)
    ang = pos[:, :, None] * inv_freq
    return jnp.cos(ang), jnp.sin(ang)


def apply_axial_rope(x, cos, sin):
    b, n, h, d = x.shape
    xs = x.reshape(b, n, h, 2, 2, d // 4)
    xa, xb = xs[..., 0, :], xs[..., 1, :]
    cc, ss = cos[None, :, None], sin[None, :, None]
    out = jnp.stack([xa * cc - xb * ss, xb * cc + xa * ss], axis=-2)
    return out.reshape(b, n, h, d).astype(x.dtype)


def context_attention(q, k, v, sink=None):
    b, m, hq, d = q.shape
    hk = k.shape[2]
    g = hq // hk
    qg = q.reshape(b, m, hk, g, d)
    s = jnp.einsum('bqhgd,bkhd->bhgqk', qg, k).astype(F32) * (d ** -0.5)
    if sink is not None:
        s_sink = jnp.broadcast_to(sink.reshape(hk, g)[None, :, :, None, None].astype(F32), s.shape[:-1] + (1,))
        s = jnp.concatenate([s, s_sink], axis=-1)
    p = jax.nn.softmax(s, axis=-1)
    if sink is not None:
        p = p[..., :-1]
    o = jnp.einsum('bhgqk,bkhd->bqhgd', p.astype(v.dtype), v)
    return o.reshape(b, m, hq * d)


def neighbourhood_attention(q, k, v, k_ctx, v_ctx, rpb):
    b, n, h, d = q.shape
    rows = n // GRID_W
    wr = min(NA_WIN_ROWS, rows)
    r = jnp.arange(rows)
    r0 = jnp.clip(r - wr // 2, 0, rows - wr)
    key_rows = r0[:, None] + jnp.arange(wr)[None, :]
    qr = q.reshape(b, rows, GRID_W, h, d)
    kg = k.reshape(b, rows, GRID_W, h, d)[:, key_rows]
    vg = v.reshape(b, rows, GRID_W, h, d)[:, key_rows]
    col = jnp.arange(GRID_W)
    c0 = jnp.clip(col - NA_WIN_COLS // 2, 0, GRID_W - NA_WIN_COLS)
    col_ok = (col[None, :] >= c0[:, None]) & (col[None, :] < c0[:, None] + NA_WIN_COLS)
    dc = jnp.clip(col[None, :] - col[:, None] + NA_WIN_COLS - 1, 0, 2 * NA_WIN_COLS - 2)
    dr = key_rows - r[:, None] + NA_WIN_ROWS - 1
    bias = rpb[:, dr[:, None, :, None], dc[None, :, None, :]].astype(F32)
    scale = d ** -0.5
    s_loc = jnp.einsum('brqhd,brikhd->bhrqik', qr, kg).astype(F32) * scale + bias[None]
    s_loc = jnp.where(col_ok[:, None, :], s_loc, NEG_INF)
    n_loc = wr * GRID_W
    s_loc = s_loc.reshape(b, h, rows, GRID_W, n_loc)
    s_ctx = jnp.einsum('brqhd,bchd->bhrqc', qr, k_ctx).astype(F32) * scale
    p = jax.nn.softmax(jnp.concatenate([s_loc, s_ctx], axis=-1), axis=-1)
    p_loc = p[..., :n_loc].reshape(b, h, rows, GRID_W, wr, GRID_W).astype(v.dtype)
    p_ctx = p[..., n_loc:].astype(v.dtype)
    o = jnp.einsum('bhrqik,brikhd->brqhd', p_loc, vg) + jnp.einsum('bhrqc,bchd->brqhd', p_ctx, v_ctx)
    return o.reshape(b, n, h * d)


def windowed_gqa(q, k, v, k_ctx, v_ctx, sink):
    b, n, hq, d = q.shape
    hk = k.shape[2]
    g = hq // hk
    nb = n // WG_BLOCK
    n_band = 2 * WG_WINDOW // WG_BLOCK + 1
    pad = ((0, 0), (WG_WINDOW, WG_WINDOW), (0, 0), (0, 0))
    kp, vp = jnp.pad(k, pad), jnp.pad(v, pad)

    def band(xp):
        parts = [xp[:, j * WG_BLOCK: j * WG_BLOCK + n].reshape(b, nb, WG_BLOCK, hk, d) for j in range(n_band)]
        return jnp.concatenate(parts, axis=2)

    kb, vb = band(kp), band(vp)
    qb = q.reshape(b, nb, WG_BLOCK, hk, g, d)
    blk = jnp.arange(nb)[:, None] * WG_BLOCK
    qpos = blk + jnp.arange(WG_BLOCK)[None, :]
    kpos = blk - WG_WINDOW + jnp.arange(n_band * WG_BLOCK)[None, :]
    valid = ((jnp.abs(qpos[:, :, None] - kpos[:, None, :]) <= WG_WINDOW)
             & (kpos[:, None, :] >= 0) & (kpos[:, None, :] < n))
    scale = d ** -0.5
    s_loc = jnp.einsum('bnqhgd,bnkhd->bhgnqk', qb, kb).astype(F32) * scale
    s_loc = jnp.where(valid, s_loc, NEG_INF)
    s_ctx = jnp.einsum('bnqhgd,bchd->bhgnqc', qb, k_ctx).astype(F32) * scale
    s_sink = jnp.broadcast_to(sink.reshape(hk, g)[None, :, :, None, None, None].astype(F32), s_ctx.shape[:-1] + (1,))
    p = jax.nn.softmax(jnp.concatenate([s_loc, s_ctx, s_sink], axis=-1), axis=-1)
    nk = n_band * WG_BLOCK
    o = (jnp.einsum('bhgnqk,bnkhd->bnqhgd', p[..., :nk].astype(v.dtype), vb)
         + jnp.einsum('bhgnqc,bchd->bnqhgd', p[..., nk:-1].astype(v.dtype), v_ctx))
    return o.reshape(b, n, hq * d)


def s5_discretise(lam_re, lam_im, log_step, b_re, b_im):
    lam_re = jnp.minimum(lam_re.astype(F32), -1e-4)
    lam_im = lam_im.astype(F32)
    step = jnp.exp(log_step.astype(F32))[:, None]
    mag = jnp.exp(lam_re * step)
    ang = lam_im * step
    a_re, a_im = mag * jnp.cos(ang), mag * jnp.sin(ang)
    den = lam_re * lam_re + lam_im * lam_im
    f_re = ((a_re - 1) * lam_re + a_im * lam_im) / den
    f_im = (a_im * lam_re - (a_re - 1) * lam_im) / den
    br, bi = b_re.astype(F32), b_im.astype(F32)
    bb_re = f_re[..., None] * br - f_im[..., None] * bi
    bb_im = f_re[..., None] * bi + f_im[..., None] * br
    return a_re, a_im, bb_re, bb_im


def complex_diag_scan(a_re, a_im, u_re, u_im, s0_re, s0_im):
    ar = jnp.broadcast_to(a_re, u_re.shape)
    ai = jnp.broadcast_to(a_im, u_re.shape)

    def combine(e1, e2):
        a1r, a1i, b1r, b1i = e1
        a2r, a2i, b2r, b2i = e2
        return (a1r * a2r - a1i * a2i, a1r * a2i + a1i * a2r,
                a2r * b1r - a2i * b1i + b2r, a2r * b1i + a2i * b1r + b2i)

    pr, pi, sr, si = lax.associative_scan(combine, (ar, ai, u_re, u_im), axis=1)
    sr = sr + pr * s0_re[:, None] - pi * s0_im[:, None]
    si = si + pr * s0_im[:, None] + pi * s0_re[:, None]
    return sr, si


def s5_states(u, init_f, init_b, disc):
    u32 = u.astype(F32)
    out = []
    for dr, (uu, init) in enumerate(((u32, init_f), (jnp.flip(u32, axis=1), init_b))):
        a_re, a_im, bb_re, bb_im = disc[dr]
        bu_re = jnp.einsum('bngh,gph->bngp', uu, bb_re)
        bu_im = jnp.einsum('bngh,gph->bngp', uu, bb_im)
        s_re, s_im = complex_diag_scan(a_re, a_im, bu_re, bu_im, init[0], init[1])
        if dr == 1:
            s_re, s_im = jnp.flip(s_re, axis=1), jnp.flip(s_im, axis=1)
        out.append((s_re, s_im))
    return out


def s5_readout(u, states, c_re, c_im, d_skip, w_glu, b_glu):
    b, n = u.shape[:2]
    y = d_skip.reshape(S5_GROUPS, S5_GROUP).astype(F32) * u.astype(F32)
    for dr in range(2):
        s_re, s_im = states[dr]
        y = (y + jnp.einsum('bngp,ghp->bngh', s_re, c_re[dr].astype(F32))
             - jnp.einsum('bngp,ghp->bngh', s_im, c_im[dr].astype(F32)))
    y = jax.nn.gelu(y.reshape(b, n, BRANCH_W))
    return (y * jax.nn.sigmoid(y @ w_glu.astype(F32) + b_glu.astype(F32))).astype(u.dtype)


def mlstm_chunkwise(q, k, v, log_i, log_f, state, want_h):
    b, h, n, d = q.shape
    nc = n // ML_CHUNK

    def chunks(t):
        return jnp.moveaxis(t.reshape(b, h, nc, ML_CHUNK, *t.shape[3:]), 2, 0)

    xs = (chunks(q), chunks(k), chunks(v), chunks(log_i), chunks(log_f))
    tri = jnp.tril(jnp.ones((ML_CHUNK, ML_CHUNK), dtype=bool))

    def step(carry, inp):
        c_prev, n_prev, m_prev = carry
        qc, kc, vc, lic, lfc = inp
        bcum = jnp.cumsum(lfc, axis=-1)
        b_last = bcum[..., -1]
        log_end = b_last[..., None] - bcum + lic
        m_new = jnp.maximum(b_last + m_prev, jnp.max(log_end, axis=-1))
        w_end = jnp.exp(log_end - m_new[..., None])
        decay = jnp.exp(b_last + m_prev - m_new)
        c_new = decay[..., None, None] * c_prev + jnp.einsum('bhs,bhsv,bhsk->bhvk', w_end, vc, kc)
        n_new = decay[..., None] * n_prev + jnp.einsum('bhs,bhsk->bhk', w_end, kc)
        if not want_h:
            return (c_new, n_new, m_new), None
        log_w = jnp.where(tri, bcum[..., :, None] - bcum[..., None, :] + lic[..., None, :], NEG_INF)
        log_inter = bcum + m_prev[..., None]
        m_t = jnp.maximum(log_inter, jnp.max(log_w, axis=-1))
        w = jnp.exp(log_w - m_t[..., None])
        inter = jnp.exp(log_inter - m_t)
        s = jnp.einsum('bhtk,bhsk->bhts', qc, kc) * w
        num = inter[..., None] * jnp.einsum('bhvk,bhtk->bhtv', c_prev, qc) + jnp.einsum('bhts,bhsv->bhtv', s, vc)
        den = inter * jnp.einsum('bhk,bhtk->bht', n_prev, qc) + jnp.sum(s, axis=-1)
        h_t = num / jnp.maximum(jnp.abs(den), jnp.exp(-m_t))[..., None]
        return (c_new, n_new, m_new), h_t

    state, hs = lax.scan(step, state, xs)
    if want_h:
        hs = jnp.moveaxis(hs, 0, 2).reshape(b, h, n, d)
    return state, hs


def mlstm_bidir(q, k, v, gate_pre, init_f, init_b, want_h):
    q, k, v = [jnp.moveaxis(t, 1, 2) for t in (q, k, v)]
    k = k * (HEAD_DIM ** -0.5)
    g = jnp.moveaxis(gate_pre, 1, 3)
    li_f, lf_f = g[:, 0], jax.nn.log_sigmoid(g[:, 1])
    li_b, lf_b = g[:, 2], jax.nn.log_sigmoid(g[:, 3])
    st_f, h_f = mlstm_chunkwise(q, k, v, li_f, lf_f, init_f, want_h)

    def fl(t):
        return jnp.flip(t, axis=2)

    st_b, h_b = mlstm_chunkwise(fl(q), fl(k), fl(v), fl(li_b), fl(lf_b), init_b, want_h)
    h = h_f + fl(h_b) if want_h else None
    return st_f, st_b, h


def mlstm_output(h, o_pre, ml_norm):
    h = jnp.moveaxis(h, 1, 2).astype(F32)
    h = h * lax.rsqrt(jnp.mean(h * h, axis=-1, keepdims=True) + EPS) * ml_norm.reshape(ML_HEADS, HEAD_DIM).astype(F32)
    b, n = h.shape[:2]
    return (h.reshape(b, n, ML_HEADS * HEAD_DIM) * jax.nn.sigmoid(o_pre.astype(F32))).astype(o_pre.dtype)


def merge_branches(branches, gate_pre, w_branch, w_out):
    br = jnp.stack(branches, axis=2)
    proj = jnp.einsum('bnim,imd->bnid', br, w_branch)
    gates = jax.nn.sigmoid(gate_pre.reshape(proj.shape).astype(F32)).astype(proj.dtype)
    return jnp.sum(gates * proj, axis=2) @ w_out


def token_mixing(hc, hx, cos, sin, w_in, na_rpb, wg_sink, s5_lam_re, s5_lam_im, s5_log_step, s5_b_re, s5_b_im,
                 s5_c_re, s5_c_im, s5_d, s5_w_glu, s5_b_glu, ml_gate_bias, ml_norm, w_branch, w_out, ctx_out):
    b, n_ctx = hc.shape[:2]
    n_lat = hx.shape[1]
    (c_naq, c_nak, c_nav, c_s5, c_wgq, c_wgk, c_wgv, c_mlq, c_mlk, c_mlv, c_mlo, c_mlg, c_gate) = \
        jnp.split(hc @ w_in, IN_SPLITS, axis=-1)
    (x_naq, x_nak, x_nav, x_s5, x_wgq, x_wgk, x_wgv, x_mlq, x_mlk, x_mlv, x_mlo, x_mlg, x_gate) = \
        jnp.split(hx @ w_in, IN_SPLITS, axis=-1)
    disc = [s5_discretise(s5_lam_re[dr], s5_lam_im[dr], s5_log_step[dr], s5_b_re[dr], s5_b_im[dr]) for dr in range(2)]

    na_kc, na_vc = split_heads(c_nak, NA_HEADS), split_heads(c_nav, NA_HEADS)
    wg_kc, wg_vc = split_heads(c_wgk, WG_KV_HEADS), split_heads(c_wgv, WG_KV_HEADS)
    u_c = c_s5.reshape(b, n_ctx, S5_GROUPS, S5_GROUP)
    zs = jnp.zeros((b, S5_GROUPS, S5_STATE), F32)
    sc_f, sc_b = s5_states(u_c, (zs, zs), (zs, zs), disc)
    ml_zero = (jnp.zeros((b, ML_HEADS, HEAD_DIM, HEAD_DIM), F32), jnp.zeros((b, ML_HEADS, HEAD_DIM), F32),
               jnp.zeros((b, ML_HEADS), F32))
    gate_c = c_mlg.reshape(b, n_ctx, 4, ML_HEADS).astype(F32) + ml_gate_bias.astype(F32)
    mst_f, mst_b, h_c = mlstm_bidir(split_heads(c_mlq, ML_HEADS), split_heads(c_mlk, ML_HEADS),
                                    split_heads(c_mlv, ML_HEADS), gate_c, ml_zero, ml_zero, ctx_out)
    y_c = None
    if ctx_out:
        na_c = context_attention(split_heads(c_naq, NA_HEADS), na_kc, na_vc)
        s5_c = s5_readout(u_c, (sc_f, sc_b), s5_c_re, s5_c_im, s5_d, s5_w_glu, s5_b_glu)
        wg_c = context_attention(split_heads(c_wgq, WG_Q_HEADS), wg_kc, wg_vc, wg_sink)
        ml_c = mlstm_output(h_c, c_mlo, ml_norm)
        y_c = merge_branches((na_c, s5_c, wg_c, ml_c), c_gate, w_branch, w_out)

    na_x = neighbourhood_attention(split_heads(x_naq, NA_HEADS), split_heads(x_nak, NA_HEADS),
                                   split_heads(x_nav, NA_HEADS), na_kc, na_vc, na_rpb)
    u_x = x_s5.reshape(b, n_lat, S5_GROUPS, S5_GROUP)
    sx = s5_states(u_x, (sc_f[0][:, -1], sc_f[1][:, -1]), (sc_b[0][:, 0], sc_b[1][:, 0]), disc)
    s5_x = s5_readout(u_x, sx, s5_c_re, s5_c_im, s5_d, s5_w_glu, s5_b_glu)
    wg_x = windowed_gqa(apply_axial_rope(split_heads(x_wgq, WG_Q_HEADS), cos, sin),
                        apply_axial_rope(split_heads(x_wgk, WG_KV_HEADS), cos, sin),
                        split_heads(x_wgv, WG_KV_HEADS), wg_kc, wg_vc, wg_sink)
    gate_x = x_mlg.reshape(b, n_lat, 4, ML_HEADS).astype(F32) + ml_gate_bias.astype(F32)
    _, _, h_x = mlstm_bidir(split_heads(x_mlq, ML_HEADS), split_heads(x_mlk, ML_HEADS),
                            split_heads(x_mlv, ML_HEADS), gate_x, mst_f, mst_b, True)
    ml_x = mlstm_output(h_x, x_mlo, ml_norm)
    y_x = merge_branches((na_x, s5_x, wg_x, ml_x), x_gate, w_branch, w_out)
    return y_c, y_x


def conv_ffn(h, w_up, conv_w, conv_b, w_down):
    u = h @ w_up
    u = lax.conv_general_dilated(u, conv_w[:, None, :].astype(u.dtype), window_strides=(1,), padding=((1, 1),),
                                 dimension_numbers=('NWC', 'WIO', 'NWC'), feature_group_count=u.shape[-1]) + conv_b
    a, g = jnp.split(u, 2, axis=-1)
    return (a * jax.nn.silu(g)) @ w_down


def setup_inputs(seed: int = 0) -> dict:
    key = jax.random.key(seed)
    ks = iter(jax.random.split(key, 40))

    def nrm(shape, scale):
        return jax.random.normal(next(ks), shape, F32) * scale

    L, D, G, P, Hg, F = DEPTH, D_MODEL, S5_GROUPS, S5_STATE, S5_GROUP, FFN_DIM
    f_bias = jnp.linspace(3.0, 6.0, ML_HEADS, dtype=F32)
    zero_h = jnp.zeros((ML_HEADS,), F32)
    gate_bias = jnp.stack([zero_h, f_bias, zero_h, f_bias])[None] + nrm((L, 4, ML_HEADS), 0.1)
    return {
        "x": nrm((BATCH, SEQ, D), 1.0),
        "c": nrm((BATCH, D), 1.0),
        "ctx": nrm((BATCH, CTX_LEN, D), 1.0),
        "c_ctx": nrm((D,), 1.0),
        "w_ada": nrm((L, D, 6 * D), D ** -0.5),
        "b_ada": nrm((L, 6 * D), 0.01),
        "g_mix_pre": 1.0 + nrm((L, D), 0.01),
        "g_mix_post": 1.0 + nrm((L, D), 0.01),
        "g_ffn_pre": 1.0 + nrm((L, D), 0.01),
        "g_ffn_post": 1.0 + nrm((L, D), 0.01),
        "w_in": nrm((L, D, IN_W), D ** -0.5),
        "na_rpb": nrm((L, NA_HEADS, 2 * NA_WIN_ROWS - 1, 2 * NA_WIN_COLS - 1), 0.1),
        "wg_sink": nrm((L, WG_Q_HEADS), 0.5),
        "s5_lam_re": -0.5 + nrm((L, 2, G, P), 0.01),
        "s5_lam_im": math.pi * jnp.arange(P, dtype=F32) + nrm((L, 2, G, P), 0.01),
        "s5_log_step": jax.random.uniform(next(ks), (L, 2, G), F32, math.log(1e-3), math.log(1e-1)),
        "s5_b_re": nrm((L, 2, G, P, Hg), (2 * Hg) ** -0.5),
        "s5_b_im": nrm((L, 2, G, P, Hg), (2 * Hg) ** -0.5),
        "s5_c_re": nrm((L, 2, G, Hg, P), P ** -0.5),
        "s5_c_im": nrm((L, 2, G, Hg, P), P ** -0.5),
        "s5_d": nrm((L, BRANCH_W), 1.0),
        "s5_w_glu": nrm((L, BRANCH_W, BRANCH_W), BRANCH_W ** -0.5),
        "s5_b_glu": nrm((L, BRANCH_W), 0.01),
        "ml_gate_bias": gate_bias,
        "ml_norm": 1.0 + nrm((L, ML_HEADS * HEAD_DIM), 0.01),
        "w_branch": nrm((L, N_BRANCH, BRANCH_W, D), BRANCH_W ** -0.5),
        "w_out": nrm((L, D, D), D ** -0.5),
        "w_up": nrm((L, D, 2 * F), D ** -0.5),
        "ffn_conv_w": nrm((L, 3, 2 * F), 3 ** -0.5),
        "ffn_conv_b": nrm((L, 2 * F), 0.01),
        "w_down": nrm((L, F, D), F ** -0.5),
    }


def reference(x, c, ctx, c_ctx, w_ada, b_ada, g_mix_pre, g_mix_post, g_ffn_pre, g_ffn_post, w_in, na_rpb, wg_sink,
              s5_lam_re, s5_lam_im, s5_log_step, s5_b_re, s5_b_im, s5_c_re, s5_c_im, s5_d, s5_w_glu, s5_b_glu,
              ml_gate_bias, ml_norm, w_branch, w_out, w_up, ffn_conv_w, ffn_conv_b, w_down):
    cos, sin = axial_rope_tables(x.shape[1])
    for l in range(DEPTH):
        ctx_out = l < DEPTH - 1
        mod_x = (jax.nn.silu(c) @ w_ada[l] + b_ada[l])[:, None, :]
        mod_c = (jax.nn.silu(c_ctx) @ w_ada[l] + b_ada[l])[None, None, :]
        sh_mx, sc_mx, gt_mx, sh_fx, sc_fx, gt_fx = jnp.split(mod_x, 6, axis=-1)
        sh_mc, sc_mc, gt_mc, sh_fc, sc_fc, gt_fc = jnp.split(mod_c, 6, axis=-1)

        hx = modulate(rms_norm(x, g_mix_pre[l]), sh_mx, sc_mx)
        hc = modulate(rms_norm(ctx, g_mix_pre[l]), sh_mc, sc_mc)
        y_c, y_x = token_mixing(hc, hx, cos, sin, w_in[l], na_rpb[l], wg_sink[l], s5_lam_re[l], s5_lam_im[l],
                                s5_log_step[l], s5_b_re[l], s5_b_im[l], s5_c_re[l], s5_c_im[l], s5_d[l],
                                s5_w_glu[l], s5_b_glu[l], ml_gate_bias[l], ml_norm[l], w_branch[l], w_out[l], ctx_out)
        x = x + gt_mx * rms_norm(y_x, g_mix_post[l])
        hx = modulate(rms_norm(x, g_ffn_pre[l]), sh_fx, sc_fx)
        x = x + gt_fx * rms_norm(conv_ffn(hx, w_up[l], ffn_conv_w[l], ffn_conv_b[l], w_down[l]), g_ffn_post[l])

        if ctx_out:
            ctx = ctx + gt_mc * rms_norm(y_c, g_mix_post[l])
            hc = modulate(rms_norm(ctx, g_ffn_pre[l]), sh_fc, sc_fc)
            ctx = ctx + gt_fc * rms_norm(conv_ffn(hc, w_up[l], ffn_conv_w[l], ffn_conv_b[l], w_down[l]), g_ffn_post[l])
    return x
```

```python
import numpy as np
from contextlib import ExitStack
import concourse.bass as bass
import concourse.mybir as mybir
from concourse.bass_utils import run_bass_kernel_spmd

F32 = mybir.dt.float32
BF16 = mybir.dt.bfloat16
AF = mybir.ActivationFunctionType
ALU = mybir.AluOpType
AX = mybir.AxisListType

FULL = dict(D=2048, SEQ=4096, CTX=256, F=5632, L=2, B=2)
NCORES = 8
HD = 128

COMPUTE = ("pe", "act", "dve", "pool")
NDMA_SEM = 24
SEM_ROLL = 30000


class Sched:
    def __init__(self, nc, stack):
        self.nc = nc; self.stack = stack
        self.streams = {e: [] for e in COMPUTE + ("sp",)}
        self.esem = {}; self.ecnt = {}; self.nsem = 0
        for e in COMPUTE:
            self._new_esem(e)
        self.dsem = [stack.enter_context(nc.semaphore(f"d{i}")) for i in range(NDMA_SEM)]
        self.dval = [0] * NDMA_SEM
        self.dnext = 0
        self.seen = {e: {} for e in self.streams}
        self.lastw = {}; self.reads = {}
        self.engs = {"pe": nc.tensor, "act": nc.scalar, "dve": nc.vector, "pool": nc.gpsimd, "sp": nc.sync}
        self.count = {e: 0 for e in self.streams}

    def barrier(self):
        toks = [(self.esem[e], self.ecnt[e], e) for e in COMPUTE if self.ecnt[e] > 0]
        toks += [(self.dsem[i], self.dval[i], "dma") for i in range(NDMA_SEM) if self.dval[i] > 0]
        for q in self.streams:
            for (sem, val, teng) in toks:
                if teng == q or self.seen[q].get(sem, 0) >= val:
                    continue
                self.engs[q].wait_ge(sem, val); self.seen[q][sem] = val

    def _new_esem(self, e):
        self.nsem += 1
        self.esem[e] = self.stack.enter_context(self.nc.semaphore(f"e_{e}_{self.nsem}"))
        self.ecnt[e] = 0

    def _need(self, eng, tok, waits):
        if tok is None:
            return
        sem, val, teng = tok
        if teng == "pe" and eng == "pe":
            return
        if self.seen[eng].get(sem, 0) >= val:
            return
        waits[sem] = max(waits.get(sem, 0), val)

    def op(self, eng, fn, reads=(), writes=()):
        is_dma = eng == "dma"
        q = "sp" if is_dma else eng
        waits = {}
        for k in reads:
            self._need(q, self.lastw.get(k), waits)
        for k in writes:
            self._need(q, self.lastw.get(k), waits)
            for t in self.reads.get(k, ()):
                self._need(q, t, waits)
        if is_dma:
            i = self.dnext; self.dnext = (self.dnext + 1) % NDMA_SEM
            sem = self.dsem[i]
            if self.dval[i] > 0:
                self._need(q, (sem, self.dval[i], "dma"), waits)
            self.dval[i] += 16
            tok = (sem, self.dval[i], "dma"); inc = 16
        else:
            if self.ecnt[eng] >= SEM_ROLL:
                self._new_esem(eng)
            self.ecnt[eng] += 1
            sem = self.esem[eng]
            tok = (sem, self.ecnt[eng], eng); inc = 1
        for s, v in waits.items():
            self.seen[q][s] = v
        eobj = self.engs[q]
        for s, v in waits.items():
            eobj.wait_ge(s, v)
        fn(eobj).then_inc(sem, inc)
        self.count[q] += 1
        for k in writes:
            self.lastw[k] = tok; self.reads[k] = []
        for k in reads:
            if k not in writes:
                self.reads.setdefault(k, []).append(tok)
        return tok

    def finish(self, final_keys):
        waits = {}
        for k in final_keys:
            self._need("sp", self.lastw.get(k), waits)
        for s_, v in waits.items():
            self.engs["sp"].wait_ge(s_, v)
        return dict(self.count)


def dma_groups(S, K, g, mk, reads=(), writes=()):
    for k0 in range(0, K, g):
        k1 = min(K, k0 + g)
        S.op("dma", (lambda e, k0=k0, k1=k1: mk(e, k0, k1)), reads=list(reads), writes=list(writes))


N_LAUNCH = [0]


def launch(nc, in_maps):
    N_LAUNCH[0] += 1
    res = run_bass_kernel_spmd(nc, in_maps, core_ids=list(range(NCORES)))
    return res.results


def chunked(a, p=128):
    K, N = a.shape
    return np.ascontiguousarray(a.reshape(K // p, p, N).transpose(1, 0, 2))


def build_p0(cfg):
    D, L = cfg["D"], cfg["L"]
    KD = D // 128
    NW = 6 * D // NCORES
    nc = bass.Bass("TRN2", target_bir_lowering=False)
    cv = nc.dram_tensor("cv", [128, KD, 128], F32, kind="ExternalInput").ap()
    wa = nc.dram_tensor("wa", [128, L * KD, NW], F32, kind="ExternalInput").ap()
    ba = nc.dram_tensor("ba", [128, L * NW], F32, kind="ExternalInput").ap()
    out = nc.dram_tensor("mod", [128, L * NW], F32, kind="ExternalOutput").ap()
    CW = 512
    with ExitStack() as st:
        S = Sched(nc, st)
        sb = lambda n, s, d: st.enter_context(nc.sbuf_tensor(n, s, d))
        cvt = sb("cvt", [128, KD, 128], F32); sg = sb("sg", [128, KD, 128], F32)
        cvb = sb("cvb", [128, KD, 128], BF16)
        bat = sb("bat", [128, L * NW], F32); res = sb("res", [128, L * NW], F32)
        w32 = [sb(f"w32_{i}", [128, NW], F32) for i in range(2)]
        w16 = [sb(f"w16_{i}", [128, NW], BF16) for i in range(2)]
        ps = [st.enter_context(nc.psum_tensor(f"ps{i}", [128, CW], F32)) for i in range(4)]
        S.op("dma", lambda e: e.dma_start(out=cvt[:], in_=cv), writes=["cvt"])
        S.op("dma", lambda e: e.dma_start(out=bat[:], in_=ba), writes=["bat"])
        S.op("act", lambda e: e.activation(out=sg[:], in_=cvt[:], func=AF.Sigmoid), reads=["cvt"], writes=["sg"])
        S.op("dve", lambda e: e.tensor_mul(cvb[:], sg[:], cvt[:]), reads=["sg", "cvt"], writes=["cvb"])
        ncw = (NW + CW - 1) // CW
        for l in range(L):
            for k in range(KD):
                i = (l * KD + k) % 2
                S.op("dma", lambda e, i=i, l=l, k=k: e.dma_start(out=w32[i][:], in_=wa[:, l * KD + k, :]),
                     writes=[f"w32_{i}"])
                S.op("pool" if k % 2 else "dve", lambda e, i=i: e.tensor_copy(w16[i][:], w32[i][:]),
                     reads=[f"w32_{i}"], writes=[f"w16_{i}"])
                for j in range(ncw):
                    c0, c1 = j * CW, min(NW, (j + 1) * CW)
                    S.op("pe", lambda e, i=i, j=j, k=k, c0=c0, c1=c1: e.matmul(
                        ps[j][:, 0:c1 - c0], cvb[:, k, :], w16[i][:, c0:c1], start=(k == 0), stop=(k == KD - 1)),
                        reads=[f"w16_{i}", "cvb"], writes=[f"ps{j}"])
            for j in range(ncw):
                c0, c1 = j * CW, min(NW, (j + 1) * CW)
                S.op("dve", lambda e, j=j, c0=c0, c1=c1, l=l: e.tensor_add(
                    res[:, l * NW + c0:l * NW + c1], ps[j][:, 0:c1 - c0], bat[:, l * NW + c0:l * NW + c1]),
                    reads=[f"ps{j}", "bat"], writes=["res"])
        S.op("dma", lambda e: e.dma_start(out=out, in_=res[:]), reads=["res"], writes=["out"])
        S.finish(["out"])
    return nc


def run_p0(cfg, c, c_ctx, w_ada, b_ada):
    D, L = cfg["D"], cfg["L"]
    NW = 6 * D // NCORES
    cv3 = np.zeros((D, 128), np.float32); cv3[:, 0] = c[0]; cv3[:, 1] = c[1]; cv3[:, 2] = c_ctx
    cv = chunked(cv3)
    maps = []
    for core in range(NCORES):
        sl = slice(core * NW, (core + 1) * NW)
        wa = np.concatenate([chunked(w_ada[l][:, sl]) for l in range(L)], axis=1)
        ba = np.concatenate([b_ada[l][sl] for l in range(L)])[None, :].repeat(128, axis=0)
        maps.append({"cv": cv, "wa": np.ascontiguousarray(wa), "ba": np.ascontiguousarray(ba)})
    res = launch(build_p0(cfg), maps)
    mod = np.zeros((L, 3, 6 * D), np.float32)
    for core in range(NCORES):
        r = res[core]["mod"][0:3].reshape(3, L, NW)
        for l in range(L):
            mod[l][:, core * NW:(core + 1) * NW] = r[:, l]
    return mod


NFM, NTM = 9, 5
OFF = dict(naq=0, nak=512, nav=1024, s5=1536, wgq=2048, wgk=2560, wgv=2816, mlq=3072, mlk=3584,
           mlv=4096, mlo=4608, mlg=5120, gate=5136)


def p1_weight_cols(w_in_l, h):
    D = w_in_l.shape[0]
    sl = lambda name, i: w_in_l[:, OFF[name] + 128 * i: OFF[name] + 128 * (i + 1)]
    gch = np.zeros((D, 128), np.float32)
    for typ in range(4):
        gch[:, 32 * typ] = w_in_l[:, OFF["mlg"] + 4 * typ + h]
    cols = [sl("naq", h), sl("nak", h), sl("s5", h), sl("wgq", h), sl("wgk", h // 2), sl("mlq", h), sl("mlk", h),
            sl("mlo", h), gch, sl("nav", h), sl("wgv", h // 2), sl("mlv", h), sl("mlk", h), sl("mlo", h)]
    return np.concatenate(cols, axis=1)


def token_tiles(cfg):
    tiles = []
    t = 0
    for n, is_ctx in ((cfg["CTX"], True), (cfg["SEQ"], False)):
        e = t + n
        while t < e:
            nt = min(512, e - t); tiles.append((t, nt, is_ctx)); t += nt
    return tiles


class PsumPool:
    def __init__(self, nc, st, n=6):
        self.t = [st.enter_context(nc.psum_tensor(f"psum{i}", [128, 512], F32)) for i in range(n)]
        self.i = 0

    def get(self):
        i = self.i; self.i = (self.i + 1) % len(self.t)
        return self.t[i], f"psum{i}"


def emit_inproj(nc, st, S, PS, cfg, xT, gpre, modv, win, projF, projT, gatesF, hxO, eps=1e-6):
    D = cfg["D"]; KD = D // 128; T = cfg["CTX"] + cfg["SEQ"]
    NC_ = (NFM + NTM) * 128
    sb = lambda n, s, d: st.enter_context(nc.sbuf_tensor(n, s, d))
    ones = sb("ones", [128, 128], BF16)
    S.op("dve", lambda e: e.memset(ones[:], 1.0), writes=["ones"])
    g_t = sb("g_t", [128, KD], F32); mv = sb("mv", [128, KD, 4], F32)
    S.op("dma", lambda e: e.dma_start(out=g_t[:], in_=gpre), writes=["g_t"])
    S.op("dma", lambda e: e.dma_start(out=mv[:], in_=modv), writes=["mv"])
    gs = [sb(f"gs{i}", [128, KD], F32) for i in range(2)]
    for i in range(2):
        S.op("dve", lambda e, i=i: e.tensor_scalar(gs[i][:], mv[:, :, 2 * i], 1.0, None, ALU.add),
             reads=["mv"], writes=[f"gs{i}"])
        S.op("dve", lambda e, i=i: e.tensor_mul(gs[i][:], gs[i][:], g_t[:]), reads=["g_t", f"gs{i}"], writes=[f"gs{i}"])
    w16 = sb("w16", [128, KD, NC_], BF16)
    w32 = [sb(f"w32_{i}", [128, NC_], F32) for i in range(2)]
    for k in range(KD):
        i = k % 2
        S.op("dma", lambda e, i=i, k=k: e.dma_start(out=w32[i][:], in_=win[:, k, :]), writes=[f"w32_{i}"])
        S.op("pool" if k % 2 else "dve", lambda e, i=i, k=k: e.tensor_copy(w16[:, k, :], w32[i][:]),
             reads=[f"w32_{i}"], writes=[("w16", k)])
    xt = sb("xt", [128, KD, 512], F32); sq = sb("sq", [128, KD, 512], BF16)
    hx = [sb(f"hx{i}", [128, KD, 512], BF16) for i in range(2)]
    rstd = sb("rstd", [128, 512], F32); tmp = [sb(f"tmp{i}", [128, 512], F32) for i in range(2)]
    stF = [sb(f"stF{i}", [128, 512], BF16) for i in range(3)]
    stT = [sb(f"stT{i}", [128, NTM * 128], BF16) for i in range(2)]
    gst = sb("gst", [128, 512], F32)
    nF = nT = 0
    for ti, (t0, nt, is_ctx) in enumerate(token_tiles(cfg)):
        hb = hx[ti % 2]; hk = f"hx{ti % 2}"; c = 1 if is_ctx else 0
        dma_groups(S, KD, 4, lambda e, k0, k1, t0=t0, nt=nt: e.dma_start(out=xt[:, k0:k1, 0:nt], in_=xT[:, k0:k1, t0:t0 + nt]), writes=["xt"])
        S.op("act", lambda e, nt=nt: e.activation(out=sq[:, :, 0:nt], in_=xt[:, :, 0:nt], func=AF.Square),
             reads=["xt"], writes=["sq"])
        ps, pk = PS.get()
        for k in range(KD):
            S.op("pe", lambda e, k=k, nt=nt, ps=ps: e.matmul(ps[:, 0:nt], ones[:], sq[:, k, 0:nt],
                 start=(k == 0), stop=(k == KD - 1)), reads=["ones", "sq"], writes=[pk])
        S.op("act", lambda e, nt=nt, ps=ps: e.activation(out=rstd[:, 0:nt], in_=ps[:, 0:nt], func=AF.Sqrt,
             scale=1.0 / D, bias=eps), reads=[pk], writes=["rstd"])
        S.op("dve", lambda e, nt=nt: e.reciprocal(rstd[:, 0:nt], rstd[:, 0:nt]), reads=["rstd"], writes=["rstd"])
        for k in range(KD):
            tb = tmp[k % 2]; tk = f"tmp{k % 2}"
            S.op("dve", lambda e, k=k, nt=nt, tb=tb: e.tensor_mul(tb[:, 0:nt], xt[:, k, 0:nt], rstd[:, 0:nt]),
                 reads=["xt", "rstd"], writes=[tk])
            S.op("act", lambda e, k=k, nt=nt, tb=tb, hb=hb, c=c: e.activation(out=hb[:, k, 0:nt], in_=tb[:, 0:nt],
                 func=AF.Identity, scale=gs[c][:, k:k + 1], bias=mv[:, k, 2 * c + 1:2 * c + 2]),
                 reads=[tk, f"gs{c}", "mv"], writes=[(hk, k)])
        dma_groups(S, KD, 4, lambda e, k0, k1, hb=hb, t0=t0, nt=nt: e.dma_start(out=hxO[:, k0:k1, t0:t0 + nt], in_=hb[:, k0:k1, 0:nt]),
                   reads=[(hk, k) for k in range(KD)], writes=[("hxO", ti)])
        for j in range(NFM):
            ps, pk = PS.get()
            for k in range(KD):
                S.op("pe", lambda e, j=j, k=k, nt=nt, ps=ps, hb=hb: e.matmul(ps[:, 0:nt], w16[:, k, j * 128:(j + 1) * 128],
                     hb[:, k, 0:nt], start=(k == 0), stop=(k == KD - 1)), reads=[("w16", k), (hk, k)], writes=[pk])
            sF = stF[nF % 3]; sk = f"stF{nF % 3}"; nF += 1
            S.op("act" if j % 2 else "dve", (lambda e, nt=nt, ps=ps, sF=sF: e.activation(out=sF[:, 0:nt], in_=ps[:, 0:nt], func=AF.Copy))
                 if j % 2 else (lambda e, nt=nt, ps=ps, sF=sF: e.tensor_copy(sF[:, 0:nt], ps[:, 0:nt])), reads=[pk], writes=[sk])
            S.op("dma", lambda e, j=j, t0=t0, nt=nt, sF=sF: e.dma_start(out=projF[j, :, t0:t0 + nt], in_=sF[:, 0:nt]),
                 reads=[sk], writes=[("projF", j, ti)])
            if j == NFM - 1:
                S.op("dve", lambda e, nt=nt, ps=ps: e.tensor_copy(gst[:, 0:nt], ps[:, 0:nt]), reads=[pk], writes=["gst"])
                S.op("dma", lambda e, t0=t0, nt=nt: e.dma_start(out=gatesF[:, t0:t0 + nt], in_=gst[:, 0:nt]), reads=["gst"], writes=[("gatesF", ti)])
        for s_ in range(nt // 128):
            sT = stT[nT % 2]; sk = f"stT{nT % 2}"; nT += 1
            for (c0, c1) in ((0, 384), (384, NTM * 128)):
                ps, pk = PS.get()
                for k in range(KD):
                    S.op("pe", lambda e, k=k, s_=s_, ps=ps, hb=hb, c0=c0, c1=c1: e.matmul(ps[:, 0:c1 - c0], hb[:, k, s_ * 128:(s_ + 1) * 128],
                         w16[:, k, NFM * 128 + c0:NFM * 128 + c1], start=(k == 0), stop=(k == KD - 1)), reads=[("w16", k), (hk, k)], writes=[pk])
                S.op("dve", lambda e, ps=ps, sT=sT, c0=c0, c1=c1: e.tensor_copy(sT[:, c0:c1], ps[:, 0:c1 - c0]), reads=[pk], writes=[(sk, c0)])
            r0 = t0 + s_ * 128
            S.op("dma", lambda e, r0=r0, sT=sT: e.dma_start(out=projT[r0:r0 + 128, :], in_=sT[:]),
                 reads=[(sk, 0), (sk, 384)], writes=[("projT", r0 // 128)])


GRID_W = 64
ROPE_BASE = 10000.0
NEG = -1e30


def rope_tables_fm(n_tok):
    t = np.arange(n_tok)
    pos = np.stack([t // GRID_W, t % GRID_W], 0).astype(np.float32)
    inv = (ROPE_BASE ** (-np.arange(32, dtype=np.float32) / 32)).astype(np.float32)
    ang = pos[:, None, :] * inv[None, :, None]
    cos = np.repeat(np.cos(ang)[:, None], 2, axis=1).reshape(128, n_tok)
    sin = np.repeat(np.sin(ang)[:, None], 2, axis=1).reshape(128, n_tok)
    return cos.astype(np.float32), sin.astype(np.float32)


def rope_perm_T():
    P = np.zeros((128, 128), np.float32)
    for half in range(2):
        for f in range(32):
            a = half * 64 + f; b = a + 32
            P[a, b] = -1.0; P[b, a] = 1.0
    return np.ascontiguousarray(P.T)


def wg_band_mask():
    qi = np.arange(128)[:, None]; kj = np.arange(384)[None, :]
    return np.where((kj - qi >= 0) & (kj - qi <= 256), 0.0, NEG).astype(np.float32)


def emit_rope(nc, st, S, PS, buf, bkey, ntok_ctx, ntok_lat, cosT, sinT, PT, name):
    sb = lambda n, s_, d: st.enter_context(nc.sbuf_tensor(n, s_, d))
    t1 = sb(name + "_t1", [128, 512], F32); t2 = sb(name + "_t2", [128, 512], F32)
    for t0 in range(0, ntok_lat, 512):
        nt = min(512, ntok_lat - t0); a = ntok_ctx + t0
        ps, pk = PS.get()
        S.op("pe", lambda e, ps=ps, a=a, nt=nt: e.matmul(ps[:, 0:nt], PT[:], buf[:, a:a + nt], start=True, stop=True),
             reads=[bkey, "ropeP"], writes=[pk])
        S.op("dve", lambda e, a=a, nt=nt, t0=t0: e.tensor_mul(t1[:, 0:nt], buf[:, a:a + nt], cosT[:, t0:t0 + nt]),
             reads=[bkey, "ropeC"], writes=[name + "_t1"])
        S.op("dve", lambda e, ps=ps, nt=nt, t0=t0: e.tensor_mul(t2[:, 0:nt], ps[:, 0:nt], sinT[:, t0:t0 + nt]),
             reads=[pk, "ropeS"], writes=[name + "_t2"])
        S.op("dve", lambda e, a=a, nt=nt: e.tensor_add(buf[:, a:a + nt], t1[:, 0:nt], t2[:, 0:nt]),
             reads=[name + "_t1", name + "_t2"], writes=[bkey])


class AttnScratch:
    def __init__(self, nc, st, nkmax, name, psT):
        sb = lambda n, s_, d: st.enter_context(nc.sbuf_tensor(name + n, s_, d))
        self.name = name
        self.z = sb("z", [128, nkmax], F32); self.p = sb("p", [128, nkmax], BF16)
        self.stat = sb("stat", [128, 8], F32)
        self.pT = [sb(f"pT{i}", [128, 128], BF16) for i in range(3)]
        self.o = [sb(f"o{i}", [128, 128], BF16) for i in range(2)]
        self.ident = sb("ident", [128, 128], BF16)
        self.psT = psT; self.pskey = "attpsT"
        self.n = 0


def emit_attn_tile(nc, S, PS, A, qbuf, qkey, kbuf, kkey, vbuf, vkey, tq, chunks, sink, out_ap, out_key, scale):
    nm = A.name; Z, P, ST = A.z, A.p, A.stat
    c0 = 0
    for (ks, n, tbl, tkey) in chunks:
        for s0 in range(0, n, 512):
            sn = min(512, n - s0)
            ps, pk = PS.get()
            S.op("pe", lambda e, ps=ps, ks=ks, s0=s0, sn=sn: e.matmul(ps[:, 0:sn], qbuf[:, tq:tq + 128],
                 kbuf[:, ks + s0:ks + s0 + sn], start=True, stop=True), reads=[qkey, kkey], writes=[pk])
            if tbl is not None:
                S.op("dve", lambda e, ps=ps, c=c0 + s0, sn=sn, tbl=tbl, s0=s0: e.scalar_tensor_tensor(
                    Z[:, c:c + sn], ps[:, 0:sn], scale, tbl[:, s0:s0 + sn], ALU.mult, ALU.add),
                    reads=[pk, tkey], writes=[nm + "z"])
            else:
                S.op("dve", lambda e, ps=ps, c=c0 + s0, sn=sn: e.tensor_scalar(Z[:, c:c + sn], ps[:, 0:sn], scale, None, ALU.mult),
                     reads=[pk], writes=[nm + "z"])
        c0 += n
    nk = c0
    S.op("dve", lambda e: e.reduce_max(ST[:, 0:1], Z[:, 0:nk], AX.X), reads=[nm + "z"], writes=[nm + "st"])
    if sink is not None:
        S.op("dve", lambda e: e.tensor_max(ST[:, 0:1], ST[:, 0:1], sink[:, 0:1]), reads=[nm + "st", "sink"], writes=[nm + "st"])
    S.op("dve", lambda e: e.tensor_scalar(ST[:, 1:2], ST[:, 0:1], -1.0, None, ALU.mult), reads=[nm + "st"], writes=[nm + "st"])
    S.op("act", lambda e: e.activation(out=P[:, 0:nk], in_=Z[:, 0:nk], func=AF.Exp, bias=ST[:, 1:2], scale=1.0,
         accum_out=ST[:, 2:3]), reads=[nm + "z", nm + "st"], writes=[nm + "p", nm + "st"])
    if sink is not None:
        S.op("act", lambda e: e.activation(out=ST[:, 3:4], in_=sink[:, 0:1], func=AF.Exp, bias=ST[:, 1:2], scale=1.0),
             reads=["sink", nm + "st"], writes=[nm + "st"])
        S.op("dve", lambda e: e.tensor_add(ST[:, 2:3], ST[:, 2:3], ST[:, 3:4]), reads=[nm + "st"], writes=[nm + "st"])
    S.op("dve", lambda e: e.reciprocal(ST[:, 4:5], ST[:, 2:3]), reads=[nm + "st"], writes=[nm + "st"])
    S.op("dve", lambda e: e.tensor_scalar(P[:, 0:nk], P[:, 0:nk], ST[:, 4:5], None, ALU.mult),
         reads=[nm + "p", nm + "st"], writes=[nm + "p"])
    po, pok = PS.get()
    blocks = []
    c0 = 0
    for (ks, n, tbl, tkey) in chunks:
        for b0 in range(0, n, 128):
            blocks.append((c0 + b0, (ks + b0) // 128))
        c0 += n
    for bi, (pc, vt) in enumerate(blocks):
        i = A.n % 2; j = A.n % 3; A.n += 1
        S.op("pe", lambda e, i=i, pc=pc: e.transpose(A.psT[i][:], P[:, pc:pc + 128], A.ident[:]),
             reads=[nm + "p", nm + "ident"], writes=[f"attpsT{i}"])
        S.op("act" if bi % 2 else "dve",
             (lambda e, i=i, j=j: e.activation(out=A.pT[j][:], in_=A.psT[i][:], func=AF.Copy)) if bi % 2 else
             (lambda e, i=i, j=j: e.tensor_copy(A.pT[j][:], A.psT[i][:])), reads=[f"attpsT{i}"], writes=[f"{nm}pT{j}"])
        S.op("pe", lambda e, j=j, vt=vt, bi=bi: e.matmul(po[:, 0:128], vbuf[:, vt, :], A.pT[j][:],
             start=(bi == 0), stop=(bi == len(blocks) - 1)), reads=[vkey, f"{nm}pT{j}"], writes=[pok])
    oi = (tq // 128) % 2
    S.op("act", lambda e, oi=oi: e.activation(out=A.o[oi][:], in_=po[:, 0:128], func=AF.Copy), reads=[pok], writes=[f"{nm}o{oi}"])
    S.op("dma", lambda e, oi=oi: e.dma_start(out=out_ap, in_=A.o[oi][:]), reads=[f"{nm}o{oi}"], writes=[out_key])


def emit_wg(nc, st, S, PS, cfg, projF, projT, consts, brT, with_ctx_out):
    C, N = cfg["CTX"], cfg["SEQ"]; T = C + N
    sb = lambda n, s_, d: st.enter_context(nc.sbuf_tensor(n, s_, d))
    q = sb("wg_q", [128, T], BF16); k = sb("wg_k", [128, T], BF16); v = sb("wg_v", [128, T // 128, 128], BF16)
    S.op("dma", lambda e: e.dma_start(out=q[:], in_=projF[3]), reads=[("projF", 3, ti) for ti in range(len(token_tiles(cfg)))], writes=["wg_q"])
    S.op("dma", lambda e: e.dma_start(out=k[:], in_=projF[4]), reads=[("projF", 4, ti) for ti in range(len(token_tiles(cfg)))], writes=["wg_k"])
    dma_groups(S, T // 128, 8, lambda e, n0, n1: e.dma_start(out=v[:, n0:n1, :], in_=projT[n0 * 128:n1 * 128, 128:256].rearrange("(n p) d -> p n d", p=128)),
               reads=[("projT", r) for r in range(T // 128)], writes=["wg_v"])
    cs_ = {"cos": sb("c_cos", [128, N], F32), "sin": sb("c_sin", [128, N], F32), "PT": sb("c_PT", [128, 128], BF16),
           "wgmask": sb("c_wgm", [128, 384], F32), "sink": sb("c_sink", [128, 1], F32)}
    for key, k2 in (("cos", "ropeC"), ("sin", "ropeS"), ("PT", "ropeP"), ("wgmask", "wgmask"), ("sink", "sink")):
        S.op("dma", lambda e, key=key: e.dma_start(out=cs_[key][:], in_=consts[key]), writes=[k2])
    consts = dict(consts, **cs_)
    A = AttnScratch(nc, st, 384 + C, "attw", consts["psT"])
    S.op("dma", lambda e: e.dma_start(out=A.ident[:], in_=consts["ident_dram"]), writes=["attwident"])
    emit_rope(nc, st, S, PS, q, "wg_q", C, N, consts["cos"], consts["sin"], consts["PT"], "rq")
    emit_rope(nc, st, S, PS, k, "wg_k", C, N, consts["cos"], consts["sin"], consts["PT"], "rk")
    scale = HD ** -0.5
    mask = consts["wgmask"]
    for q0 in range(0, N, 128):
        lo = max(0, q0 - 128); hi = min(N, q0 + 256)
        tbl = mask[:, (lo - (q0 - 128)):(hi - (q0 - 128))]
        chunks = [(C + lo, hi - lo, tbl, "wgmask"), (0, C, None, None)]
        emit_attn_tile(nc, S, PS, A, q, "wg_q", k, "wg_k", v, "wg_v", C + q0, chunks, consts["sink"],
                       brT[2, :, C + q0:C + q0 + 128], ("brT", 2, (C + q0) // 128), scale)
    if with_ctx_out:
        for q0 in range(0, C, 128):
            emit_attn_tile(nc, S, PS, A, q, "wg_q", k, "wg_k", v, "wg_v", q0, [(0, C, None, None)], consts["sink"],
                           brT[2, :, q0:q0 + 128], ("brT", 2, q0 // 128), scale)


NA_ROWS, NA_COLS, NA_BAND = 8, 16, 10


def na_tile_plan(n_lat):
    R_ = n_lat // GRID_W
    plan = []
    for r in range(0, R_, 2):
        r0 = [int(np.clip(rr - NA_ROWS // 2, 0, R_ - NA_ROWS)) for rr in (r, r + 1)]
        lo = min(r0); lo -= lo % 2; lo = min(lo, R_ - NA_BAND)
        assert lo >= 0 and max(r0) + NA_ROWS <= lo + NA_BAND
        plan.append((r, lo, tuple(r0)))
    return plan


def na_bias_tables(rpb_h, n_lat):
    col = np.arange(GRID_W)
    c0 = np.clip(col - NA_COLS // 2, 0, GRID_W - NA_COLS)
    col_ok = (col[None, :] >= c0[:, None]) & (col[None, :] < c0[:, None] + NA_COLS)
    dc = np.clip(col[None, :] - col[:, None] + NA_COLS - 1, 0, 2 * NA_COLS - 2)
    uniq, index, seen = [], [], {}
    for (r, lo, r0s) in na_tile_plan(n_lat):
        key = (r - lo, r0s[0] - lo, r0s[1] - lo)
        if key not in seen:
            tb = np.full((2, GRID_W, NA_BAND, GRID_W), NEG, np.float32)
            for qr in range(2):
                for kr in range(NA_BAND):
                    krow = lo + kr
                    if r0s[qr] <= krow < r0s[qr] + NA_ROWS:
                        dr = krow - (r + qr) + NA_ROWS - 1
                        tb[qr, :, kr, :] = np.where(col_ok, rpb_h[dr][dc], NEG)
            seen[key] = len(uniq); uniq.append(tb.reshape(128, NA_BAND * GRID_W))
        index.append(seen[key])
    return np.stack(uniq), index


def emit_na(nc, st, S, PS, cfg, projF, projT, consts, brT, with_ctx_out, na_tab, na_index, ident_key_src):
    C, N = cfg["CTX"], cfg["SEQ"]; T = C + N
    sb = lambda n, s_, d: st.enter_context(nc.sbuf_tensor(n, s_, d))
    q = sb("na_q", [128, T], BF16); k = sb("na_k", [128, T], BF16); v = sb("na_v", [128, T // 128, 128], BF16)
    allF = lambda j: [("projF", j, ti) for ti in range(len(token_tiles(cfg)))]
    S.op("dma", lambda e: e.dma_start(out=q[:], in_=projF[0]), reads=allF(0), writes=["na_q"])
    S.op("dma", lambda e: e.dma_start(out=k[:], in_=projF[1]), reads=allF(1), writes=["na_k"])
    dma_groups(S, T // 128, 8, lambda e, n0, n1: e.dma_start(out=v[:, n0:n1, :], in_=projT[n0 * 128:n1 * 128, 0:128].rearrange("(n p) d -> p n d", p=128)),
               reads=[("projT", r) for r in range(T // 128)], writes=["na_v"])
    U = na_tab.shape[0]
    tabs = [sb(f"na_tab{u}", [128, NA_BAND * GRID_W], F32) for u in range(U)]
    for u in range(U):
        S.op("dma", lambda e, u=u: e.dma_start(out=tabs[u][:], in_=na_tab[u]), writes=[f"natab{u}"])
    A = AttnScratch(nc, st, NA_BAND * GRID_W + C, "attn", consts["psT"])
    S.op("dma", lambda e: e.dma_start(out=A.ident[:], in_=consts["ident_dram"]), writes=["attnident"])
    scale = HD ** -0.5
    for ti, (r, lo, r0s) in enumerate(na_tile_plan(N)):
        u = na_index[ti]; tq = C + r * GRID_W
        chunks = [(C + lo * GRID_W, NA_BAND * GRID_W, tabs[u], f"natab{u}"), (0, C, None, None)]
        emit_attn_tile(nc, S, PS, A, q, "na_q", k, "na_k", v, "na_v", tq, chunks, None,
                       brT[0, :, tq:tq + 128], ("brT", 0, tq // 128), scale)
    if with_ctx_out:
        for q0 in range(0, C, 128):
            emit_attn_tile(nc, S, PS, A, q, "na_q", k, "na_k", v, "na_v", q0, [(0, C, None, None)], None,
                           brT[0, :, q0:q0 + 128], ("brT", 0, q0 // 128), scale)


S5_L = 512
TWO_PI = float(2 * np.pi)


MAGIC = 12582912.0


def emit_sin(S, out, x, shift, tmp, reads, writes, tkey):
    S.op("dve", lambda e: e.tensor_scalar(tmp, x, float(shift), 1.0 / TWO_PI, ALU.add, ALU.mult), reads=reads, writes=[tkey])
    S.op("dve", lambda e: e.tensor_scalar_add(tmp, tmp, MAGIC), reads=[tkey], writes=[tkey])
    S.op("dve", lambda e: e.tensor_scalar_add(tmp, tmp, -MAGIC), reads=[tkey], writes=[tkey])
    S.op("dve", lambda e: e.scalar_tensor_tensor(tmp, tmp, -TWO_PI, x, ALU.mult, ALU.add), reads=[tkey] + list(reads), writes=[tkey])
    S.op("dve", lambda e: e.tensor_scalar(tmp, tmp, float(shift), None, ALU.add), reads=[tkey], writes=[tkey])
    S.op("dve", lambda e: e.tensor_scalar(tmp, tmp, float(np.pi), -float(np.pi), ALU.min, ALU.max), reads=[tkey], writes=[tkey])
    S.op("act", lambda e: e.activation(out=out, in_=tmp, func=AF.Sin), reads=[tkey], writes=writes)


def s5_host_params(p, l, h):
    out = {}
    G0 = 8 * h
    col = np.zeros((2, 4, 128, 4), np.float32)
    Bp = np.zeros((2, 4, 2, 128, 128), np.float32)
    CT = np.zeros((2, 4, 2, 128, 128), np.float32)
    for dr in range(2):
        for st_ in range(4):
            for gl in range(2):
                g = G0 + 2 * st_ + gl; rows = slice(64 * gl, 64 * gl + 64); cols = slice(16 * (2 * st_ + gl), 16 * (2 * st_ + gl) + 16)
                col[dr, st_, rows, 0] = p["s5_lam_re"][l, dr, g]; col[dr, st_, rows, 1] = p["s5_lam_im"][l, dr, g]
                col[dr, st_, rows, 2] = p["s5_log_step"][l, dr, g]
                Bp[dr, st_, 0, rows, cols] = p["s5_b_re"][l, dr, g]; Bp[dr, st_, 1, rows, cols] = p["s5_b_im"][l, dr, g]
                CT[dr, st_, 0, rows, cols] = p["s5_c_re"][l, dr, g].T; CT[dr, st_, 1, rows, cols] = p["s5_c_im"][l, dr, g].T
    out["s5col"] = col.transpose(2, 0, 1, 3).reshape(128, 32).copy()
    out["s5B"] = Bp.transpose(3, 0, 1, 2, 4).reshape(128, 16 * 128).copy()
    out["s5C"] = CT.transpose(3, 0, 1, 2, 4).reshape(128, 16 * 128).copy()
    out["s5d"] = np.ascontiguousarray(p["s5_d"][l][128 * h:128 * h + 128, None]).astype(np.float32)
    out["s5ramp"] = np.tile(np.arange(S5_L, dtype=np.float32)[None, :], (128, 1))
    return out


def emit_s5(nc, st, S, PS, cfg, projF, brT, d_in, ident_f32_key):
    C, N = cfg["CTX"], cfg["SEQ"]; T = C + N
    sb = lambda n, s_, d: st.enter_context(nc.sbuf_tensor("s5t_" + n, s_, d))
    L = S5_L
    col = sb("col", [128, 32], F32); Braw = sb("Braw", [128, 16 * 128], F32); Craw = sb("Craw", [128, 16 * 128], F32)
    dsk = sb("d", [128, 1], F32); ramp = sb("ramp", [128, L], F32); idf = sb("idf", [128, 128], F32)
    for t_, src, k in ((col, d_in["s5col"], "s5col"), (Braw, d_in["s5B"], "s5B"), (Craw, d_in["s5C"], "s5C"),
                       (dsk, d_in["s5d"], "s5d"), (ramp, d_in["s5ramp"], "s5ramp"), (idf, d_in["identf"], "s5idf")):
        S.op("dma", lambda e, t_=t_, src=src: e.dma_start(out=t_[:], in_=src), writes=[k])
    u = sb("u", [128, T], BF16)
    S.op("dma", lambda e: e.dma_start(out=u[:], in_=projF[2]), reads=[("projF", 2, ti) for ti in range(len(token_tiles(cfg)))], writes=["s5u"])
    W = sb("W", [128, 8, 20], F32)
    cosT = sb("cos", [128, 8, L], F32); sinT = sb("sin", [128, 8, L], F32); rT = sb("r", [128, 8, L], F32)
    BT = sb("BT", [128, 16, 128], BF16)
    CTb = sb("CT", [128, 16, 128], BF16)
    tmpB = sb("tmpB", [128, 2, 128], F32)
    ang = sb("ang", [128, L], F32); ang2 = sb("ang2", [128, L], F32)
    K_ = "s5W"
    def dv(fn, reads=(), writes=()):
        S.op("dve", fn, reads=list(reads), writes=list(writes))
    for dr in range(2):
        for s_ in range(4):
            i = dr * 4 + s_; w = lambda c, i=i: W[:, i, c:c + 1]
            lr, li, ls = (col[:, i * 4 + c:i * 4 + c + 1] for c in range(3))
            dv(lambda e, w=w, lr=lr: e.tensor_scalar_min(w(0), lr, -1e-4), ["s5col"], [K_])
            S.op("act", lambda e, w=w, ls=ls: e.activation(out=w(1), in_=ls, func=AF.Exp), reads=["s5col"], writes=[K_])
            dv(lambda e, w=w: e.tensor_mul(w(11), w(0), w(1)), [K_], [K_])
            S.op("act", lambda e, w=w: e.activation(out=w(2), in_=w(11), func=AF.Exp), reads=[K_], writes=[K_])
            dv(lambda e, w=w, li=li: e.tensor_mul(w(3), li, w(1)), [K_, "s5col"], [K_])
            emit_sin(S, w(10), w(3), 0.0, w(12), [K_], [K_], K_)
            emit_sin(S, w(9), w(3), 0.5 * np.pi, w(12), [K_], [K_], K_)
            dv(lambda e, w=w: e.tensor_mul(w(5), w(2), w(10)), [K_], [K_])
            dv(lambda e, w=w: e.tensor_mul(w(4), w(2), w(9)), [K_], [K_])
            dv(lambda e, w=w: e.tensor_scalar_add(w(4), w(4), -1.0), [K_], [K_])
            dv(lambda e, w=w: e.tensor_mul(w(6), w(0), w(0)), [K_], [K_])
            dv(lambda e, w=w, li=li: e.tensor_mul(w(12), li, li), ["s5col"], [K_])
            dv(lambda e, w=w: e.tensor_add(w(6), w(6), w(12)), [K_], [K_])
            dv(lambda e, w=w: e.reciprocal(w(6), w(6)), [K_], [K_])
            dv(lambda e, w=w: e.tensor_mul(w(7), w(4), w(0)), [K_], [K_])
            dv(lambda e, w=w, li=li: e.tensor_mul(w(12), w(5), li), [K_, "s5col"], [K_])
            dv(lambda e, w=w: e.tensor_add(w(7), w(7), w(12)), [K_], [K_])
            dv(lambda e, w=w: e.tensor_mul(w(7), w(7), w(6)), [K_], [K_])
            dv(lambda e, w=w: e.tensor_mul(w(8), w(5), w(0)), [K_], [K_])
            dv(lambda e, w=w, li=li: e.tensor_mul(w(12), w(4), li), [K_, "s5col"], [K_])
            dv(lambda e, w=w: e.tensor_sub(w(8), w(8), w(12)), [K_], [K_])
            dv(lambda e, w=w: e.tensor_mul(w(8), w(8), w(6)), [K_], [K_])
            Bre = Braw[:, (i * 2) * 128:(i * 2 + 1) * 128]; Bim = Braw[:, (i * 2 + 1) * 128:(i * 2 + 2) * 128]
            dv(lambda e, w=w, Bim=Bim: e.tensor_scalar(tmpB[:, 1, :], Bim, w(8), None, ALU.mult), [K_, "s5B"], ["s5tB"])
            dv(lambda e, w=w, Bre=Bre: e.scalar_tensor_tensor(tmpB[:, 0, :], Bre, w(7), tmpB[:, 1, :], ALU.mult, ALU.subtract), [K_, "s5B", "s5tB"], ["s5tB0"])
            dv(lambda e, w=w, Bre=Bre: e.tensor_scalar(tmpB[:, 1, :], Bre, w(8), None, ALU.mult), [K_, "s5B", "s5tB0"], ["s5tB"])
            dv(lambda e, w=w, Bim=Bim: e.scalar_tensor_tensor(tmpB[:, 1, :], Bim, w(7), tmpB[:, 1, :], ALU.mult, ALU.add), [K_, "s5B", "s5tB"], ["s5tB"])
            for ri in range(2):
                ps, pk = PS.get()
                S.op("pe", lambda e, ps=ps, ri=ri: e.transpose(ps[:, 0:128], tmpB[:, ri, :], idf[:]), reads=["s5tB", "s5tB0", "s5idf"], writes=[pk])
                dv(lambda e, ps=ps, i=i, ri=ri: e.tensor_copy(BT[:, i * 2 + ri, :], ps[:, 0:128]), [pk], [("s5BT", i)])
            dv(lambda e, i=i: e.tensor_copy(CTb[:, i * 2, :], Craw[:, (i * 2) * 128:(i * 2 + 1) * 128]), ["s5C"], [("s5CT", i)])
            dv(lambda e, i=i: e.tensor_scalar(CTb[:, i * 2 + 1, :], Craw[:, (i * 2 + 1) * 128:(i * 2 + 2) * 128], -1.0, None, ALU.mult), ["s5C"], [("s5CT", i)])
            dv(lambda e, i=i: e.tensor_scalar(rT[:, i, :], ramp[:], 0.0, W[:, i, 2:3], ALU.mult, ALU.add), ["s5ramp", K_], [("s5tab", i)])
            dv(lambda e, i=i: e.tensor_scalar(ang2[:], ramp[:], W[:, i, 3:4], None, ALU.mult), ["s5ramp", K_], ["s5ang2"])
            emit_sin(S, sinT[:, i, :], ang2[:], 0.0, ang[:], ["s5ang2"], [("s5tab", i)], "s5ang")
            emit_sin(S, cosT[:, i, :], ang2[:], 0.5 * np.pi, ang[:], ["s5ang2"], [("s5tab", i)], "s5ang")
    yacc = sb("yacc", [128, T], F32)
    dv(lambda e: e.tensor_scalar(yacc[:], u[:], dsk[:, 0:1], None, ALU.mult), ["s5u", "s5d"], ["s5y"])
    wre = sb("wre", [128, L], F32); wim = sb("wim", [128, L], F32); t1 = sb("t1", [128, L], F32)
    qre = sb("qre", [128, L], F32); qim = sb("qim", [128, L], F32)
    sre = [sb(f"sre{j}", [128, L], BF16) for j in range(4)]; sim_ = [sb(f"sim{j}", [128, L], BF16) for j in range(4)]
    stt = sb("state", [128, 8, 8], F32)
    dv(lambda e: e.memset(stt[:], 0.0), [], ["s5st"])
    segs = [(0, C), (C, N)]
    for dr in range(2):
        order = []
        for (a0, n) in segs:
            chunks = [(a0 + c0, min(L, n - c0)) for c0 in range(0, n, L)]
            order += chunks if dr == 0 else chunks[::-1]
        for (t0, nt) in order:
            R = (lambda ap: ap) if dr == 0 else (lambda ap: ap[:, ::-1])
            for s_ in range(4):
                i = dr * 4 + s_; KS = ("s5st", i)
                pr, prk = PS.get(); pi, pik = PS.get()
                S.op("pe", lambda e, pr=pr, i=i, t0=t0, nt=nt: e.matmul(pr[:, 0:nt], BT[:, i * 2, :], u[:, t0:t0 + nt], start=True, stop=True), reads=[("s5BT", i), "s5u"], writes=[prk])
                S.op("pe", lambda e, pi=pi, i=i, t0=t0, nt=nt: e.matmul(pi[:, 0:nt], BT[:, i * 2 + 1, :], u[:, t0:t0 + nt], start=True, stop=True), reads=[("s5BT", i), "s5u"], writes=[pik])
                cs, sn = cosT[:, i, 0:nt], sinT[:, i, 0:nt]
                vr, vi = R(pr[:, 0:nt]), R(pi[:, 0:nt])
                dv(lambda e, vi=vi, sn=sn, nt=nt: e.tensor_mul(t1[:, 0:nt], vi, sn), [pik, ("s5tab", i)], ["s5t1"])
                dv(lambda e, vr=vr, cs=cs, nt=nt: e.tensor_mul(wre[:, 0:nt], vr, cs), [prk, ("s5tab", i)], ["s5wre"])
                dv(lambda e, nt=nt: e.tensor_add(wre[:, 0:nt], wre[:, 0:nt], t1[:, 0:nt]), ["s5t1", "s5wre"], ["s5wre"])
                dv(lambda e, vr=vr, sn=sn, nt=nt: e.tensor_mul(t1[:, 0:nt], vr, sn), [prk, ("s5tab", i), "s5wre"], ["s5t1"])
                dv(lambda e, vi=vi, cs=cs, nt=nt: e.tensor_mul(wim[:, 0:nt], vi, cs), [pik, ("s5tab", i)], ["s5wim"])
                dv(lambda e, nt=nt: e.tensor_sub(wim[:, 0:nt], wim[:, 0:nt], t1[:, 0:nt]), ["s5t1", "s5wim"], ["s5wim"])
                c1, s1 = W[:, i, 9:10], W[:, i, 10:11]
                x = lambda c, i=i: stt[:, i, c:c + 1]
                dv(lambda e, x=x, s1=s1: e.tensor_mul(x(4), x(1), s1), [KS, K_], [KS])
                dv(lambda e, x=x, c1=c1: e.scalar_tensor_tensor(x(2), x(0), c1, x(4), ALU.mult, ALU.subtract), [KS, K_], [KS])
                dv(lambda e, x=x, s1=s1: e.tensor_mul(x(4), x(0), s1), [KS, K_], [KS])
                dv(lambda e, x=x, c1=c1: e.scalar_tensor_tensor(x(3), x(1), c1, x(4), ALU.mult, ALU.add), [KS, K_], [KS])
                dv(lambda e, x=x, i=i, nt=nt: e.tensor_tensor_scan(qre[:, 0:nt], rT[:, i, 0:nt], wre[:, 0:nt], x(2), ALU.mult, ALU.add), [("s5tab", i), "s5wre", KS], ["s5qre"])
                dv(lambda e, x=x, i=i, nt=nt: e.tensor_tensor_scan(qim[:, 0:nt], rT[:, i, 0:nt], wim[:, 0:nt], x(3), ALU.mult, ALU.add), [("s5tab", i), "s5wim", KS], ["s5qim"])
                so_r, so_i = R(sre[s_][:, 0:nt]), R(sim_[s_][:, 0:nt])
                dv(lambda e, sn=sn, nt=nt: e.tensor_mul(t1[:, 0:nt], qim[:, 0:nt], sn), ["s5qim", ("s5tab", i), "s5wim"], ["s5t1"])
                dv(lambda e, cs=cs, nt=nt: e.tensor_mul(wre[:, 0:nt], qre[:, 0:nt], cs), ["s5qre", ("s5tab", i)], ["s5wre"])
                dv(lambda e, so_r=so_r, nt=nt: e.tensor_sub(so_r, wre[:, 0:nt], t1[:, 0:nt]), ["s5wre", "s5t1"], [f"s5sre{s_}"])
                dv(lambda e, sn=sn, nt=nt: e.tensor_mul(t1[:, 0:nt], qre[:, 0:nt], sn), ["s5qre", ("s5tab", i), f"s5sre{s_}"], ["s5t1"])
                dv(lambda e, cs=cs, nt=nt: e.tensor_mul(wim[:, 0:nt], qim[:, 0:nt], cs), ["s5qim", ("s5tab", i)], ["s5wim"])
                dv(lambda e, so_i=so_i, nt=nt: e.tensor_add(so_i, wim[:, 0:nt], t1[:, 0:nt]), ["s5wim", "s5t1"], [f"s5sim{s_}"])
                lc, ls_ = cosT[:, i, nt - 1:nt], sinT[:, i, nt - 1:nt]
                dv(lambda e, x=x, ls_=ls_, nt=nt: e.tensor_mul(x(4), qim[:, nt - 1:nt], ls_), ["s5qim", ("s5tab", i), KS], [KS])
                dv(lambda e, x=x, lc=lc, nt=nt: e.scalar_tensor_tensor(x(0), qre[:, nt - 1:nt], lc, x(4), ALU.mult, ALU.subtract), ["s5qre", ("s5tab", i), KS], [KS])
                dv(lambda e, x=x, ls_=ls_, nt=nt: e.tensor_mul(x(4), qre[:, nt - 1:nt], ls_), ["s5qre", ("s5tab", i), KS], [KS])
                dv(lambda e, x=x, lc=lc, nt=nt: e.scalar_tensor_tensor(x(1), qim[:, nt - 1:nt], lc, x(4), ALU.mult, ALU.add), ["s5qim", ("s5tab", i), KS], [KS])
            py, pyk = PS.get()
            for s_ in range(4):
                i = dr * 4 + s_
                S.op("pe", lambda e, py=py, i=i, s_=s_, nt=nt: e.matmul(py[:, 0:nt], CTb[:, i * 2, :], sre[s_][:, 0:nt], start=(s_ == 0), stop=False), reads=[("s5CT", i), f"s5sre{s_}"], writes=[pyk])
                S.op("pe", lambda e, py=py, i=i, s_=s_, nt=nt: e.matmul(py[:, 0:nt], CTb[:, i * 2 + 1, :], sim_[s_][:, 0:nt], start=False, stop=(s_ == 3)), reads=[("s5CT", i), f"s5sim{s_}"], writes=[pyk])
            dv(lambda e, py=py, t0=t0, nt=nt: e.tensor_add(yacc[:, t0:t0 + nt], yacc[:, t0:t0 + nt], py[:, 0:nt]), [pyk, "s5y"], ["s5y"])
    g1 = sb("g1", [128, 512], F32); g2 = sb("g2", [128, 512], F32); go = [sb(f"go{j}", [128, 512], BF16) for j in range(2)]
    for ci, t0 in enumerate(range(0, T, 512)):
        nt = min(512, T - t0); y = yacc[:, t0:t0 + nt]; gb = go[ci % 2]
        dv(lambda e, y=y, nt=nt: e.tensor_mul(g1[:, 0:nt], y, y), ["s5y"], ["s5g1"])
        dv(lambda e, nt=nt: e.tensor_scalar(g1[:, 0:nt], g1[:, 0:nt], 0.044715, 1.0, ALU.mult, ALU.add), ["s5g1"], ["s5g1"])
        dv(lambda e, y=y, nt=nt: e.tensor_mul(g1[:, 0:nt], g1[:, 0:nt], y), ["s5g1", "s5y"], ["s5g1"])
        S.op("act", lambda e, nt=nt: e.activation(out=g2[:, 0:nt], in_=g1[:, 0:nt], func=AF.Sigmoid, scale=1.5957691216057308), reads=["s5g1"], writes=["s5g2"])
        dv(lambda e, y=y, nt=nt, gb=gb: e.tensor_mul(gb[:, 0:nt], g2[:, 0:nt], y), ["s5g2", "s5y"], [f"s5go{ci % 2}"])
        S.op("dma", lambda e, t0=t0, nt=nt, gb=gb: e.dma_start(out=brT[1, :, t0:t0 + nt], in_=gb[:, 0:nt]), reads=[f"s5go{ci % 2}"], writes=[("brT", 1, "c", ci)])
    return [("brT", 1, "c", ci) for ci in range((T + 511) // 512)]


ML_L = 128


def ml_host_consts(p, l, h):
    tri = np.tril(np.ones((128, 128), bool))
    out = {"mltril": np.where(tri, 0.0, NEG).astype(np.float32), "mltriu": np.where(tri.T, 0.0, NEG).astype(np.float32),
           "mlsel": np.zeros((4, 128, 128), np.float32), "mlones": np.ones((128, 128), np.float32),
           "mlbias": np.tile(p["ml_gate_bias"][l][:, h][None, :], (128, 1)).astype(np.float32),
           "mlnorm": np.tile(p["ml_norm"][l][128 * h:128 * h + 128][None, :], (128, 1)).astype(np.float32)}
    for typ in range(4):
        out["mlsel"][typ, 32 * typ, :] = 1.0
    out["mlsel"] = out["mlsel"].transpose(1, 0, 2).reshape(128, 512).copy()
    return out


def emit_ml(nc, st, S, PS, cfg, projF, projT, gatesF, brT, d_in, consts, with_ctx_out):
    C, N = cfg["CTX"], cfg["SEQ"]; T = C + N; L = ML_L; NTt = T // 128
    sb = lambda n, s_, d: st.enter_context(nc.sbuf_tensor("ml_" + n, s_, d))
    dv = lambda fn, r=(), w=(): S.op("dve", fn, reads=list(r), writes=list(w))
    ac = lambda fn, r=(), w=(): S.op("act", fn, reads=list(r), writes=list(w))
    allF = lambda j: [("projF", j, ti) for ti in range(len(token_tiles(cfg)))]
    allT = [("projT", r) for r in range(NTt)]
    q = sb("q", [128, T], BF16); k = sb("k", [128, T], BF16); g32 = sb("g32", [128, T], F32)
    vx = sb("vx", [128, NTt, 132], BF16); kt = sb("kt", [128, NTt, 128], BF16); ot = sb("ot", [128, NTt, 128], BF16)
    S.op("dma", lambda e: e.dma_start(out=q[:], in_=projF[5]), reads=allF(5), writes=["mlq"])
    S.op("dma", lambda e: e.dma_start(out=k[:], in_=projF[6]), reads=allF(6), writes=["mlk"])
    S.op("dma", lambda e: e.dma_start(out=g32[:], in_=gatesF), reads=[("gatesF", ti) for ti in range(len(token_tiles(cfg)))], writes=["mlg"])
    tm = lambda c0, n0, n1: projT[n0 * 128:n1 * 128, c0:c0 + 128].rearrange("(n p) d -> p n d", p=128)
    dv(lambda e: e.memset(vx[:], 1.0), [], ["mlvx"])
    dma_groups(S, NTt, 8, lambda e, n0, n1: e.dma_start(out=vx[:, n0:n1, 0:128], in_=tm(256, n0, n1)), reads=allT + ["mlvx"], writes=["mlvx"])
    dma_groups(S, NTt, 8, lambda e, n0, n1: e.dma_start(out=kt[:, n0:n1, :], in_=tm(384, n0, n1)), reads=allT, writes=["mlkt"])
    dma_groups(S, NTt, 8, lambda e, n0, n1: e.dma_start(out=ot[:, n0:n1, :], in_=tm(512, n0, n1)), reads=allT, writes=["mlot"])
    tril = sb("tril", [128, 128], F32); triu = sb("triu", [128, 128], F32); sel = sb("sel", [128, 512], F32)
    ones = sb("ones", [128, 128], F32); bias = sb("bias", [128, 4], F32); nrm = sb("nrm", [128, 128], F32)
    idf = sb("idf", [128, 128], F32); idb = sb("idb", [128, 128], BF16)
    for t_, kname in ((tril, "mltril"), (triu, "mltriu"), (sel, "mlsel"), (ones, "mlones"), (bias, "mlbias"), (nrm, "mlnorm"), (idf, "identf")):
        S.op("dma", lambda e, t_=t_, kname=kname: e.dma_start(out=t_[:], in_=d_in[kname]), writes=["c_" + kname])
    S.op("dma", lambda e: e.dma_start(out=idb[:], in_=consts["ident_dram"]), writes=["c_idb"])
    nb = sb("nb", [128, 4], F32)
    dv(lambda e: e.tensor_scalar(nb[:], bias[:], -1.0, None, ALU.mult), ["c_mlbias"], ["mlnb"])
    hacc = sb("hacc", [128, NTt, 128], F32)
    dv(lambda e: e.memset(hacc[:], 0.0), [], ["mlh"])
    li = sb("li", [128, L], F32); lf = sb("lf", [128, L], F32); bc = sb("bc", [128, L], F32); gr = sb("gr", [128, L], F32)
    lw = sb("lw", [128, L], F32); wm = sb("wm", [128, L], F32); sm = sb("sm", [128, L], BF16); smT = sb("smT", [128, L], BF16)
    we = sb("we", [128, L], F32); t2 = sb("t2", [128, 132], F32); num = sb("num", [128, 132], F32); vw = sb("vw", [128, 132], BF16)
    hh = sb("hh", [128, 128], F32)
    c = sb("c", [128, 16], F32)
    Cst = sb("Cst", [128, 132], F32); Cbf = sb("Cbf", [128, 132], BF16); mprev = sb("mprev", [128, 1], F32)
    KC = "mlc"
    for dr in range(2):
        mask, mkey = (tril, "c_mltril") if dr == 0 else (triu, "c_mltriu")
        RV = (lambda ap: ap) if dr == 0 else (lambda ap: ap[:, ::-1])
        dv(lambda e: e.memset(Cst[:], 0.0), [], ["mlCst"]); dv(lambda e: e.memset(Cbf[:], 0.0), [], ["mlCbf"])
        dv(lambda e: e.memset(mprev[:], 0.0), [], ["mlm"])
        chunks = []
        for (a0, n) in ((0, C), (C, N)):
            cl = list(range(a0, a0 + n, L)); chunks += cl if dr == 0 else cl[::-1]
        for t0 in chunks:
            want_h = with_ctx_out or t0 >= C
            ci = t0 // 128
            for typ, dst, dk_ in ((2 * dr, li, "mlli"), (2 * dr + 1, lf, "mllf")):
                ps, pk = PS.get()
                S.op("pe", lambda e, ps=ps, typ=typ, t0=t0: e.matmul(ps[:, 0:L], sel[:, typ * 128:(typ + 1) * 128], g32[:, t0:t0 + L], start=True, stop=True), reads=["c_mlsel", "mlg"], writes=[pk])
                if typ % 2 == 0:
                    dv(lambda e, ps=ps, typ=typ: e.tensor_scalar(li[:], ps[:, 0:L], bias[:, typ:typ + 1], None, ALU.add), [pk, "c_mlbias"], ["mlli"])
                else:
                    ac(lambda e, ps=ps, typ=typ: e.activation(out=lf[:], in_=ps[:, 0:L], func=AF.Exp, scale=-1.0, bias=nb[:, typ:typ + 1]), [pk, "mlnb"], ["mllf"])
                    ac(lambda e: e.activation(out=lf[:], in_=lf[:], func=AF.Ln, bias=1.0), ["mllf"], ["mllf"])
                    dv(lambda e: e.tensor_scalar(lf[:], lf[:], -1.0, None, ALU.mult), ["mllf"], ["mllf"])
            dv(lambda e: e.tensor_tensor_scan(RV(bc[:]), RV(ones[:, 0:L]), RV(lf[:]), 0.0, ALU.mult, ALU.add), ["mllf", "c_mlones"], ["mlbc"])
            dv(lambda e: e.tensor_sub(gr[:], li[:], bc[:]), ["mlli", "mlbc"], ["mlgr"])
            col = lambda j: c[:, j:j + 1]
            dv(lambda e: e.tensor_mul(lw[:], bc[:], idf[:]), ["mlbc", "c_identf"], ["mllw"])
            dv(lambda e: e.reduce_sum(col(0), lw[:], AX.X), ["mllw"], [KC])
            blast = bc[:, L - 1:L] if dr == 0 else bc[:, 0:1]
            dv(lambda e, blast=blast: e.tensor_copy(col(6), blast), ["mlbc"], [KC])
            if want_h:
                dv(lambda e: e.scalar_tensor_tensor(lw[:], gr[:], col(0), mask[:], ALU.add, ALU.add), ["mlgr", KC, mkey, "mllw"], ["mllw"])
                dv(lambda e: e.reduce_max(col(1), lw[:], AX.X), ["mllw"], [KC])
                dv(lambda e: e.tensor_add(col(2), col(0), mprev[:]), [KC, "mlm"], [KC])
                dv(lambda e: e.tensor_max(col(3), col(2), col(1)), [KC], [KC])
                dv(lambda e: e.tensor_scalar(col(4), col(3), -1.0, None, ALU.mult), [KC], [KC])
                ac(lambda e: e.activation(out=wm[:], in_=lw[:], func=AF.Exp, bias=col(4)), ["mllw", KC], ["mlwm"])
                ac(lambda e: e.activation(out=col(5), in_=col(2), func=AF.Exp, bias=col(4)), [KC], [KC])
                ac(lambda e: e.activation(out=col(10), in_=col(3), func=AF.Exp, scale=-1.0), [KC], [KC])
                pq, pqk = PS.get()
                S.op("pe", lambda e, pq=pq, t0=t0: e.matmul(pq[:, 0:L], q[:, t0:t0 + L], k[:, t0:t0 + L], start=True, stop=True), reads=["mlq", "mlk"], writes=[pqk])
                dv(lambda e, pq=pq: e.scalar_tensor_tensor(sm[:], pq[:, 0:L], HD ** -0.5, wm[:], ALU.mult, ALU.mult), [pqk, "mlwm"], ["mlsm"])
                S.op("pe", lambda e: e.transpose(consts["psT"][0][:], sm[:], idb[:]), reads=["mlsm", "c_idb"], writes=["attpsT0"])
                dv(lambda e: e.tensor_copy(smT[:], consts["psT"][0][:]), ["attpsT0"], ["mlsmT"])
                p2, p2k = PS.get(); p1, p1k = PS.get()
                S.op("pe", lambda e, p2=p2, ci=ci: e.matmul(p2[:, 0:129], smT[:], vx[:, ci, 0:129], start=True, stop=True), reads=["mlsmT", "mlvx"], writes=[p2k])
                S.op("pe", lambda e, p1=p1, t0=t0: e.matmul(p1[:, 0:129], q[:, t0:t0 + L], Cbf[:, 0:129], start=True, stop=True), reads=["mlq", "mlCbf"], writes=[p1k])
                ac(lambda e, p2=p2: e.activation(out=t2[:, 0:129], in_=p2[:, 0:129], func=AF.Copy), [p2k], ["mlt2"])
                dv(lambda e, p1=p1: e.scalar_tensor_tensor(num[:, 0:129], p1[:, 0:129], col(5), t2[:, 0:129], ALU.mult, ALU.add), [p1k, KC, "mlt2"], ["mlnum"])
                dv(lambda e: e.tensor_scalar(col(11), num[:, 128:129], -1.0, None, ALU.mult), ["mlnum"], [KC])
                dv(lambda e: e.tensor_max(col(11), col(11), num[:, 128:129]), ["mlnum", KC], [KC])
                dv(lambda e: e.tensor_max(col(11), col(11), col(10)), [KC], [KC])
                dv(lambda e: e.reciprocal(col(12), col(11)), [KC], [KC])
                dv(lambda e, ci=ci: e.scalar_tensor_tensor(hacc[:, ci, :], num[:, 0:128], col(12), hacc[:, ci, :], ALU.mult, ALU.add), ["mlnum", KC, "mlh"], ["mlh"])
            dv(lambda e: e.reduce_max(col(8), gr[:], AX.X), ["mlgr"], [KC])
            dv(lambda e: e.tensor_add(col(8), col(8), col(6)), [KC], [KC])
            dv(lambda e: e.tensor_add(col(7), col(6), mprev[:]), [KC, "mlm"], [KC])
            dv(lambda e: e.tensor_sub(col(9), col(7), col(8)), [KC], [KC])
            dv(lambda e: e.tensor_max(col(7), col(7), col(8)), [KC], [KC])
            dv(lambda e: e.tensor_sub(col(14), col(6), col(7)), [KC], [KC])
            ac(lambda e: e.activation(out=we[:], in_=gr[:], func=AF.Exp, bias=col(14)), ["mlgr", KC], ["mlwe"])
            dv(lambda e: e.tensor_mul(we[:], we[:], idf[:]), ["mlwe", "c_identf"], ["mlwe"])
            dv(lambda e: e.reduce_sum(col(13), we[:], AX.X), ["mlwe"], [KC])
            dv(lambda e, ci=ci: e.tensor_scalar(vw[:, 0:129], vx[:, ci, 0:129], col(13), None, ALU.mult), ["mlvx", KC], ["mlvw"])
            pu, puk = PS.get()
            S.op("pe", lambda e, pu=pu, ci=ci: e.matmul(pu[:, 0:129], kt[:, ci, :], vw[:, 0:129], start=True, stop=True), reads=["mlkt", "mlvw"], writes=[puk])
            dv(lambda e: e.tensor_add(col(9), col(14), mprev[:]), [KC, "mlm"], [KC])
            ac(lambda e: e.activation(out=col(9), in_=col(9), func=AF.Exp), [KC], [KC])
            dv(lambda e, pu=pu: e.scalar_tensor_tensor(Cst[:, 0:129], Cst[:, 0:129], col(9), pu[:, 0:129], ALU.mult, ALU.add), ["mlCst", KC, puk], ["mlCst"])
            dv(lambda e: e.tensor_scalar(Cbf[:, 0:129], Cst[:, 0:129], HD ** -0.5, None, ALU.mult), ["mlCst"], ["mlCbf"])
            dv(lambda e: e.tensor_copy(mprev[:], col(7)), [KC], ["mlm"])
    sq = sb("sq", [128, 128], F32); sg = sb("sg", [128, 128], F32); hb = sb("hb", [128, 128], BF16)
    ob = [sb(f"ob{j}", [128, 128], BF16) for j in range(2)]
    lo_ = 0 if with_ctx_out else C // 128
    for ci in range(lo_, NTt):
        col = lambda j: c[:, j:j + 1]
        ac(lambda e, ci=ci: e.activation(out=sq[:], in_=hacc[:, ci, :], func=AF.Square, accum_out=col(0)), ["mlh"], ["mlsq", KC])
        ac(lambda e: e.activation(out=col(1), in_=col(0), func=AF.Sqrt, scale=1.0 / 128, bias=1e-6), [KC], [KC])
        dv(lambda e: e.reciprocal(col(1), col(1)), [KC], [KC])
        ac(lambda e, ci=ci: e.activation(out=sg[:], in_=ot[:, ci, :], func=AF.Sigmoid), ["mlot"], ["mlsg"])
        dv(lambda e, ci=ci: e.scalar_tensor_tensor(hh[:], hacc[:, ci, :], col(1), nrm[:], ALU.mult, ALU.mult), ["mlh", KC, "c_mlnorm"], ["mlhh"])
        dv(lambda e: e.tensor_mul(hb[:], hh[:], sg[:]), ["mlhh", "mlsg"], ["mlhb"])
        S.op("pe", lambda e: e.transpose(consts["psT"][1][:], hb[:], idb[:]), reads=["mlhb", "c_idb"], writes=["attpsT1"])
        o_ = ob[ci % 2]
        dv(lambda e, o_=o_: e.tensor_copy(o_[:], consts["psT"][1][:]), ["attpsT1"], [f"mlob{ci % 2}"])
        S.op("dma", lambda e, ci=ci, o_=o_: e.dma_start(out=brT[3, :, ci * 128:(ci + 1) * 128], in_=o_[:]), reads=[f"mlob{ci % 2}"], writes=[("brT", 3, ci)])
    return [("brT", 3, ci) for ci in range(lo_, NTt)]


def build_p1(cfg, debug_proj=False, mixers=("wg", "na"), with_ctx_out=True):
    na_index = na_bias_tables(np.zeros((2 * NA_ROWS - 1, 2 * NA_COLS - 1), np.float32), cfg["SEQ"])[1]
    D = cfg["D"]; KD = D // 128; T = cfg["CTX"] + cfg["SEQ"]
    NC_ = (NFM + NTM) * 128
    nc = bass.Bass("TRN2", target_bir_lowering=False)
    xT = nc.dram_tensor("xT", [128, KD, T], F32, kind="ExternalInput").ap()
    gpre = nc.dram_tensor("gpre", [128, KD], F32, kind="ExternalInput").ap()
    modv = nc.dram_tensor("modv", [128, KD, 4], F32, kind="ExternalInput").ap()
    win = nc.dram_tensor("win", [128, KD, NC_], F32, kind="ExternalInput").ap()
    kind = dict(kind="ExternalOutput") if debug_proj else {}
    projF = nc.dram_tensor("projF", [NFM, 128, T], BF16, **kind).ap()
    projT = nc.dram_tensor("projT", [T, NTM * 128], BF16, **kind).ap()
    gatesF = nc.dram_tensor("gatesF", [128, T], F32).ap()
    N = cfg["SEQ"]
    cos_d = nc.dram_tensor("cosT", [128, N], F32, kind="ExternalInput").ap()
    sin_d = nc.dram_tensor("sinT", [128, N], F32, kind="ExternalInput").ap()
    PT_d = nc.dram_tensor("ropePT", [128, 128], BF16, kind="ExternalInput").ap()
    id_d = nc.dram_tensor("ident", [128, 128], BF16, kind="ExternalInput").ap()
    wgm_d = nc.dram_tensor("wgmask", [128, 384], F32, kind="ExternalInput").ap()
    sink_d = nc.dram_tensor("sink", [128, 1], F32, kind="ExternalInput").ap()
    NU = len(set(na_index))
    s5_d = {"s5col": nc.dram_tensor("s5col", [128, 32], F32, kind="ExternalInput").ap(),
            "s5B": nc.dram_tensor("s5B", [128, 2048], F32, kind="ExternalInput").ap(),
            "s5C": nc.dram_tensor("s5C", [128, 2048], F32, kind="ExternalInput").ap(),
            "s5d": nc.dram_tensor("s5d", [128, 1], F32, kind="ExternalInput").ap(),
            "s5ramp": nc.dram_tensor("s5ramp", [128, S5_L], F32, kind="ExternalInput").ap(),
            "identf": nc.dram_tensor("identf", [128, 128], F32, kind="ExternalInput").ap()}
    ml_d = {"identf": s5_d["identf"]}
    for nm_, shp in (("mltril", [128, 128]), ("mltriu", [128, 128]), ("mlsel", [128, 512]), ("mlones", [128, 128]), ("mlbias", [128, 4]), ("mlnorm", [128, 128])):
        ml_d[nm_] = nc.dram_tensor(nm_, shp, F32, kind="ExternalInput").ap()
    natab_d = nc.dram_tensor("natab", [NU, 128, NA_BAND * GRID_W], F32, kind="ExternalInput").ap()
    brT = nc.dram_tensor("brT", [4, 128, T], BF16, kind="ExternalOutput").ap()
    hxO = nc.dram_tensor("hxT", [128, KD, T], BF16, kind="ExternalOutput").ap()
    with ExitStack() as st:
        S = Sched(nc, st); PS = PsumPool(nc, st)
        psT = [st.enter_context(nc.psum_tensor(f"attpsT{i}", [128, 128], BF16)) for i in range(2)]
        consts = {"cos": cos_d, "sin": sin_d, "PT": PT_d, "wgmask": wgm_d, "sink": sink_d, "ident_dram": id_d, "psT": psT}
        with ExitStack() as sst:
            emit_inproj(nc, sst, S, PS, cfg, xT, gpre, modv, win, projF, projT, gatesF, hxO)
            S.barrier()
        final = [("projF", j, ti) for j in range(NFM) for ti in range(len(token_tiles(cfg)))] + \
                [("projT", r) for r in range(T // 128)] + [("hxO", ti) for ti in range(len(token_tiles(cfg)))]
        lo_r = 0 if with_ctx_out else cfg["CTX"] // 128
        if not with_ctx_out:
            with ExitStack() as sst:
                zt = sst.enter_context(nc.sbuf_tensor("zfill", [128, cfg["CTX"]], BF16))
                S.op("dve", lambda e: e.memset(zt[:], 0.0), writes=["zfill"])
                for i in (0, 2, 3):
                    S.op("dma", lambda e, i=i: e.dma_start(out=brT[i, :, 0:cfg["CTX"]], in_=zt[:]), reads=["zfill"], writes=[("brTz", i)])
                    final.append(("brTz", i))
                S.barrier()
        if "wg" in mixers:
            with ExitStack() as sst:
                emit_wg(nc, sst, S, PS, cfg, projF, projT, consts, brT, with_ctx_out)
                S.barrier()
            final += [("brT", 2, r) for r in range(lo_r, T // 128)]
        if "na" in mixers:
            with ExitStack() as sst:
                emit_na(nc, sst, S, PS, cfg, projF, projT, consts, brT, with_ctx_out, natab_d, na_index, id_d)
                S.barrier()
            final += [("brT", 0, r) for r in range(lo_r, T // 128)]
        if "s5" in mixers:
            with ExitStack() as sst:
                final += emit_s5(nc, sst, S, PS, cfg, projF, brT, s5_d, None)
                S.barrier()
        if "ml" in mixers:
            with ExitStack() as sst:
                final += emit_ml(nc, sst, S, PS, cfg, projF, projT, gatesF, brT, ml_d, consts, with_ctx_out)
                S.barrier()
        S.finish(final)
    return nc


def p1_inputs(cfg, l, b, h, xT_full, mod, g_mix_pre, w_in, wg_sink, na_rpb, params):
    D = cfg["D"]
    mx, mc = mod[l, b], mod[l, 2]
    modv = np.stack([mx[D:2 * D], mx[0:D], mc[D:2 * D], mc[0:D]], axis=1)
    import ml_dtypes
    cos, sin = rope_tables_fm(cfg["SEQ"])
    return {"xT": chunked(xT_full), "gpre": np.ascontiguousarray(g_mix_pre[l].reshape(D // 128, 128).T),
            "modv": chunked(modv), "win": chunked(p1_weight_cols(w_in[l], h)),
            "cosT": cos, "sinT": sin, "ropePT": rope_perm_T().astype(ml_dtypes.bfloat16),
            "ident": np.eye(128, dtype=np.float32).astype(ml_dtypes.bfloat16), "wgmask": wg_band_mask(),
            "sink": np.full((128, 1), wg_sink[l][h], np.float32),
            "natab": na_bias_tables(na_rpb[l][h], cfg["SEQ"])[0],
            "identf": np.eye(128, dtype=np.float32), **s5_host_params(params, l, h), **ml_host_consts(params, l, h)}


def load_cast(nc, S, st, name, src, KC, ncols):
    w16 = st.enter_context(nc.sbuf_tensor(name, [128, KC, ncols], BF16))
    stg = [st.enter_context(nc.sbuf_tensor(f"{name}_s{i}", [128, ncols], F32)) for i in range(2)]
    for k in range(KC):
        i = k % 2
        S.op("dma", lambda e, i=i, k=k: e.dma_start(out=stg[i][:], in_=src[:, k, :]), writes=[f"{name}_s{i}"])
        S.op("pool" if k % 2 else "dve", lambda e, i=i, k=k: e.tensor_copy(w16[:, k, :], stg[i][:]), reads=[f"{name}_s{i}"], writes=[(name, k)])
    return w16


def emit_rstd(nc, S, PS, ones, src, KD, nt, D, rstd, sq, key_src, eps=1e-6):
    ksrc = list(key_src) if isinstance(key_src, (list, tuple)) and not (len(key_src) == 2 and isinstance(key_src[1], int)) else [key_src]
    S.op("act", lambda e: e.activation(out=sq[:, :, 0:nt], in_=src[:, :, 0:nt], func=AF.Square), reads=ksrc, writes=["sq"])
    ps, pk = PS.get()
    for k in range(KD):
        S.op("pe", lambda e, k=k: e.matmul(ps[:, 0:nt], ones[:], sq[:, k, 0:nt], start=(k == 0), stop=(k == KD - 1)), reads=["ones", "sq"], writes=[pk])
    S.op("act", lambda e: e.activation(out=rstd[:, 0:nt], in_=ps[:, 0:nt], func=AF.Sqrt, scale=1.0 / D, bias=eps), reads=[pk], writes=["rstd"])
    S.op("dve", lambda e: e.reciprocal(rstd[:, 0:nt], rstd[:, 0:nt]), reads=["rstd"], writes=["rstd"])


def build_p2(cfg):
    D = cfg["D"]; KD = D // 128; T = cfg["CTX"] + cfg["SEQ"]; DS = D // 4; DC = DS // 128
    nc = bass.Bass("TRN2", target_bir_lowering=False)
    hxT = nc.dram_tensor("hxT", [128, KD, T], BF16, kind="ExternalInput").ap()
    wg = nc.dram_tensor("wg", [128, KD, 4 * DS], F32, kind="ExternalInput").ap()
    brF = nc.dram_tensor("brF", [128, 16, T], BF16, kind="ExternalInput").ap()
    wb = nc.dram_tensor("wb", [128, 16, DS], F32, kind="ExternalInput").ap()
    wglu = nc.dram_tensor("wglu", [128, 4, 512], F32, kind="ExternalInput").ap()
    bglu = nc.dram_tensor("bglu", [128, 4], F32, kind="ExternalInput").ap()
    out = nc.dram_tensor("mergedT", [DC, 128, T], BF16, kind="ExternalOutput").ap()
    with ExitStack() as st:
        S = Sched(nc, st); PS = PsumPool(nc, st, 8)
        sb = lambda n, s_, d: st.enter_context(nc.sbuf_tensor(n, s_, d))
        wg16 = load_cast(nc, S, st, "wg16", wg, KD, 4 * DS)
        wb16 = load_cast(nc, S, st, "wb16", wb, 16, DS)
        wgl16 = load_cast(nc, S, st, "wgl16", wglu, 4, 512)
        bg = sb("bg", [128, 4], F32)
        S.op("dma", lambda e: e.dma_start(out=bg[:], in_=bglu), writes=["bg"])
        hx = [sb(f"hx{i}", [128, KD, 512], BF16) for i in range(2)]
        br = [sb(f"br{i}", [128, 16, 512], BF16) for i in range(2)]
        s5o = sb("s5o", [128, 4, 512], BF16); sg = sb("sg", [128, 512], F32)
        G = sb("G", [128, 512], F32); acc = sb("acc", [128, 512], F32); tmp = sb("tmp", [128, 512], F32)
        ob = [sb(f"ob{i}", [128, 512], BF16) for i in range(2)]
        no = 0
        for ti, t0 in enumerate(range(0, T, 512)):
            nt = min(512, T - t0); hb, bb = hx[ti % 2], br[ti % 2]; hk, bk = f"hx{ti % 2}", f"br{ti % 2}"
            dma_groups(S, KD, 8, lambda e, k0, k1, hb=hb, t0=t0, nt=nt: e.dma_start(out=hb[:, k0:k1, 0:nt], in_=hxT[:, k0:k1, t0:t0 + nt]), writes=[hk])
            dma_groups(S, 16, 8, lambda e, k0, k1, bb=bb, t0=t0, nt=nt: e.dma_start(out=bb[:, k0:k1, 0:nt], in_=brF[:, k0:k1, t0:t0 + nt]), writes=[bk])
            for oc in range(4):
                ps, pk = PS.get()
                for kc in range(4):
                    S.op("pe", lambda e, ps=ps, oc=oc, kc=kc, bb=bb, nt=nt: e.matmul(ps[:, 0:nt], wgl16[:, kc, oc * 128:(oc + 1) * 128], bb[:, 4 + kc, 0:nt],
                         start=(kc == 0), stop=(kc == 3)), reads=[("wgl16", kc), bk], writes=[pk])
                S.op("act", lambda e, ps=ps, oc=oc, nt=nt: e.activation(out=sg[:, 0:nt], in_=ps[:, 0:nt], func=AF.Sigmoid, bias=bg[:, oc:oc + 1]), reads=[pk, "bg"], writes=["sg"])
                S.op("dve", lambda e, oc=oc, bb=bb, nt=nt: e.tensor_mul(s5o[:, oc, 0:nt], sg[:, 0:nt], bb[:, 4 + oc, 0:nt]), reads=["sg", bk], writes=[("s5o", oc)])
            for dc in range(DC):
                for i in range(4):
                    pg, pgk = PS.get()
                    for k in range(KD):
                        S.op("pe", lambda e, pg=pg, i=i, dc=dc, k=k, hb=hb, nt=nt: e.matmul(pg[:, 0:nt], wg16[:, k, i * DS + dc * 128:i * DS + (dc + 1) * 128], hb[:, k, 0:nt],
                             start=(k == 0), stop=(k == KD - 1)), reads=[("wg16", k), hk], writes=[pgk])
                    S.op("act", lambda e, pg=pg, nt=nt: e.activation(out=G[:, 0:nt], in_=pg[:, 0:nt], func=AF.Sigmoid), reads=[pgk], writes=["G"])
                    pp, ppk = PS.get()
                    for kc in range(4):
                        rhs = (lambda kc=kc, i=i, bb=bb: s5o[:, kc, 0:nt] if i == 1 else bb[:, i * 4 + kc, 0:nt])
                        S.op("pe", lambda e, pp=pp, i=i, dc=dc, kc=kc, rhs=rhs, nt=nt: e.matmul(pp[:, 0:nt], wb16[:, i * 4 + kc, dc * 128:(dc + 1) * 128], rhs(),
                             start=(kc == 0), stop=(kc == 3)), reads=[("wb16", i * 4 + kc), bk] + [("s5o", j) for j in range(4)], writes=[ppk])
                    if i == 0:
                        S.op("dve", lambda e, pp=pp, nt=nt: e.tensor_mul(acc[:, 0:nt], G[:, 0:nt], pp[:, 0:nt]), reads=["G", ppk], writes=["acc"])
                    else:
                        S.op("dve", lambda e, pp=pp, nt=nt: e.tensor_mul(tmp[:, 0:nt], G[:, 0:nt], pp[:, 0:nt]), reads=["G", ppk], writes=["tmp"])
                        S.op("dve", lambda e, nt=nt: e.tensor_add(acc[:, 0:nt], acc[:, 0:nt], tmp[:, 0:nt]), reads=["tmp", "acc"], writes=["acc"])
                o_ = ob[no % 2]; ok_ = f"ob{no % 2}"; no += 1
                S.op("act", lambda e, o_=o_, nt=nt: e.activation(out=o_[:, 0:nt], in_=acc[:, 0:nt], func=AF.Copy), reads=["acc"], writes=[ok_])
                S.op("dma", lambda e, o_=o_, dc=dc, t0=t0, nt=nt: e.dma_start(out=out[dc, :, t0:t0 + nt], in_=o_[:, 0:nt]), reads=[ok_], writes=[("out", dc, ti)])
        S.finish([("out", dc, ti) for dc in range(DC) for ti in range((T + 511) // 512)])
    return nc


def build_matT(cfg, K, name_out="yT", out_dtype=None):
    D = cfg["D"]; T = cfg["CTX"] + cfg["SEQ"]; DS = D // 4; DC = DS // 128; KC = K // 128
    nc = bass.Bass("TRN2", target_bir_lowering=False)
    aT = nc.dram_tensor("aT", [128, KC, T], BF16, kind="ExternalInput").ap()
    w = nc.dram_tensor("w", [128, KC, DS], F32, kind="ExternalInput").ap()
    out = nc.dram_tensor(name_out, [DC, 128, T], F32, kind="ExternalOutput").ap()
    NTK = 256 if KC > 24 else 512
    with ExitStack() as st:
        S = Sched(nc, st); PS = PsumPool(nc, st, 8)
        sb = lambda n, s_, d: st.enter_context(nc.sbuf_tensor(n, s_, d))
        w16 = load_cast(nc, S, st, "w16", w, KC, DS)
        a = [sb(f"a{i}", [128, KC, NTK], BF16) for i in range(2)]
        ob = [sb(f"ob{i}", [128, NTK], F32) for i in range(2)]
        no = 0
        tiles = list(range(0, T, NTK))
        for ti, t0 in enumerate(tiles):
            nt = min(NTK, T - t0); ab, ak = a[ti % 2], f"a{ti % 2}"
            dma_groups(S, KC, 8, lambda e, k0, k1, ab=ab, t0=t0, nt=nt: e.dma_start(out=ab[:, k0:k1, 0:nt], in_=aT[:, k0:k1, t0:t0 + nt]), writes=[ak])
            for dc in range(DC):
                ps, pk = PS.get()
                for k in range(KC):
                    S.op("pe", lambda e, ps=ps, dc=dc, k=k, ab=ab, nt=nt: e.matmul(ps[:, 0:nt], w16[:, k, dc * 128:(dc + 1) * 128], ab[:, k, 0:nt],
                         start=(k == 0), stop=(k == KC - 1)), reads=[("w16", k), ak], writes=[pk])
                o_ = ob[no % 2]; ok_ = f"ob{no % 2}"; no += 1
                S.op("dve" if no % 2 else "act", (lambda e, o_=o_, ps=ps, nt=nt: e.tensor_copy(o_[:, 0:nt], ps[:, 0:nt])) if no % 2 else
                     (lambda e, o_=o_, ps=ps, nt=nt: e.activation(out=o_[:, 0:nt], in_=ps[:, 0:nt], func=AF.Copy)), reads=[pk], writes=[ok_])
                S.op("dma", lambda e, o_=o_, dc=dc, t0=t0, nt=nt: e.dma_start(out=out[dc, :, t0:t0 + nt], in_=o_[:, 0:nt]), reads=[ok_], writes=[("out", dc, ti)])
        S.finish([("out", dc, ti) for dc in range(DC) for ti in range(len(tiles))])
    return nc


def build_p5(cfg):
    D = cfg["D"]; KD = D // 128; C, N = cfg["CTX"], cfg["SEQ"]; T = C + N; DS = D // 4; DC = DS // 128
    FS = cfg["F"] // 4; FC = FS // 128
    nc = bass.Bass("TRN2", target_bir_lowering=False)
    I = lambda n, shp, dt=F32: nc.dram_tensor(n, shp, dt, kind="ExternalInput").ap()
    xT = I("xT", [128, KD, T]); yT = I("yT", [128, KD, T]); xo = I("xo", [128, DC, T]); yo = I("yo", [128, DC, T])
    vec = I("vec", [128, KD, 8]); veco = I("veco", [128, DC, 4]); wup = I("wup", [128, KD, 2 * FS]); cw = I("cw", [128, 2 * FC, 4])
    x1o = nc.dram_tensor("x1T", [DC, 128, T], F32, kind="ExternalOutput").ap()
    act = nc.dram_tensor("actT", [FC, 128, T], BF16, kind="ExternalOutput").ap()
    U = nc.dram_tensor("Uscr", [2 * FC, 128, T], F32).ap()
    tiles = token_tiles(cfg)
    with ExitStack() as st:
        S = Sched(nc, st); PS = PsumPool(nc, st, 8)
        with ExitStack() as sst:
            sb = lambda n, s_, d: sst.enter_context(nc.sbuf_tensor(n, s_, d))
            ones = sb("ones", [128, 128], BF16)
            S.op("dve", lambda e: e.memset(ones[:], 1.0), writes=["ones"])
            v = sb("v", [128, KD, 8], F32); vo = sb("vo", [128, DC, 4], F32)
            S.op("dma", lambda e: e.dma_start(out=v[:], in_=vec), writes=["v"])
            S.op("dma", lambda e: e.dma_start(out=vo[:], in_=veco), writes=["vo"])
            gg = [sb(f"gg{i}", [128, KD], F32) for i in range(2)]; gs = [sb(f"gs{i}", [128, KD], F32) for i in range(2)]
            ggo = [sb(f"ggo{i}", [128, DC], F32) for i in range(2)]
            for i in range(2):
                S.op("dve", lambda e, i=i: e.tensor_mul(gg[i][:], v[:, :, 0], v[:, :, 1 + i]), reads=["v"], writes=[f"gg{i}"])
                S.op("dve", lambda e, i=i: e.tensor_mul(ggo[i][:], vo[:, :, 0], vo[:, :, 1 + i]), reads=["vo"], writes=[f"ggo{i}"])
                S.op("dve", lambda e, i=i: e.tensor_scalar(gs[i][:], v[:, :, 4 + 2 * i], 1.0, None, ALU.add), reads=["v"], writes=[f"gs{i}"])
                S.op("dve", lambda e, i=i: e.tensor_mul(gs[i][:], gs[i][:], v[:, :, 3]), reads=["v", f"gs{i}"], writes=[f"gs{i}"])
            w16 = load_cast(nc, S, sst, "w16", wup, KD, 2 * FS)
            NT = 256
            xt = sb("xt", [128, KD, NT], F32); yt = sb("yt", [128, KD, NT], F32); sq = sb("sq", [128, KD, NT], BF16)
            xot = sb("xot", [128, DC, NT], F32); yot = sb("yot", [128, DC, NT], F32)
            hx = [sb(f"hx{i}", [128, KD, NT], BF16) for i in range(2)]
            rstd = sb("rstd", [128, NT], F32); rs2 = sb("rs2", [128, NT], F32); tmp = [sb(f"tmp{i}", [128, NT], F32) for i in range(2)]
            ub = [sb(f"ub{i}", [128, NT], F32) for i in range(3)]
            nu = 0; tix = 0
            for (T0, NTT, is_ctx) in tiles:
              for t0 in range(T0, T0 + NTT, NT):
                nt = min(NT, T0 + NTT - t0); c = 1 if is_ctx else 0; hb = hx[tix % 2]; hk = f"hx{tix % 2}"; tix += 1
                dma_groups(S, KD, 8, lambda e, k0, k1, t0=t0, nt=nt: e.dma_start(out=xt[:, k0:k1, 0:nt], in_=xT[:, k0:k1, t0:t0 + nt]), writes=[("xt", k) for k in range(KD)])
                dma_groups(S, KD, 8, lambda e, k0, k1, t0=t0, nt=nt: e.dma_start(out=yt[:, k0:k1, 0:nt], in_=yT[:, k0:k1, t0:t0 + nt]), writes=[("yt", k) for k in range(KD)])
                S.op("dma", lambda e, t0=t0, nt=nt: e.dma_start(out=xot[:, :, 0:nt], in_=xo[:, :, t0:t0 + nt]), writes=[("xot", k) for k in range(DC)])
                S.op("dma", lambda e, t0=t0, nt=nt: e.dma_start(out=yot[:, :, 0:nt], in_=yo[:, :, t0:t0 + nt]), writes=[("yot", k) for k in range(DC)])
                emit_rstd(nc, S, PS, ones, yt, KD, nt, D, rstd, sq, [("yt", k) for k in range(KD)])
                for k in range(KD):
                    S.op("dve", lambda e, k=k, nt=nt: e.tensor_mul(yt[:, k, 0:nt], yt[:, k, 0:nt], rstd[:, 0:nt]), reads=[("yt", k), "rstd"], writes=[("yt", k)])
                    S.op("dve", lambda e, k=k, nt=nt, c=c: e.scalar_tensor_tensor(xt[:, k, 0:nt], yt[:, k, 0:nt], gg[c][:, k:k + 1], xt[:, k, 0:nt], ALU.mult, ALU.add),
                         reads=[("yt", k), f"gg{c}", ("xt", k)], writes=[("xt", k)])
                for k in range(DC):
                    S.op("dve", lambda e, k=k, nt=nt: e.tensor_mul(yot[:, k, 0:nt], yot[:, k, 0:nt], rstd[:, 0:nt]), reads=[("yot", k), "rstd"], writes=[("yot", k)])
                    S.op("dve", lambda e, k=k, nt=nt, c=c: e.scalar_tensor_tensor(xot[:, k, 0:nt], yot[:, k, 0:nt], ggo[c][:, k:k + 1], xot[:, k, 0:nt], ALU.mult, ALU.add),
                         reads=[("yot", k), f"ggo{c}", ("xot", k)], writes=[("xot", k)])
                    S.op("dma", lambda e, k=k, t0=t0, nt=nt: e.dma_start(out=x1o[k, :, t0:t0 + nt], in_=xot[:, k, 0:nt]), reads=[("xot", k)], writes=[("x1out", k, t0)])
                S.op("act", lambda e, nt=nt: e.activation(out=sq[:, :, 0:nt], in_=xt[:, :, 0:nt], func=AF.Square), reads=[("xt", k) for k in range(KD)], writes=["sq"])
                ps, pk = PS.get()
                for k in range(KD):
                    S.op("pe", lambda e, k=k, ps=ps, nt=nt: e.matmul(ps[:, 0:nt], ones[:], sq[:, k, 0:nt], start=(k == 0), stop=(k == KD - 1)), reads=["ones", "sq"], writes=[pk])
                S.op("act", lambda e, ps=ps, nt=nt: e.activation(out=rs2[:, 0:nt], in_=ps[:, 0:nt], func=AF.Sqrt, scale=1.0 / D, bias=1e-6), reads=[pk], writes=["rs2"])
                S.op("dve", lambda e, nt=nt: e.reciprocal(rs2[:, 0:nt], rs2[:, 0:nt]), reads=["rs2"], writes=["rs2"])
                for k in range(KD):
                    tb = tmp[k % 2]; tk = f"tmp{k % 2}"
                    S.op("dve", lambda e, k=k, nt=nt, tb=tb: e.tensor_mul(tb[:, 0:nt], xt[:, k, 0:nt], rs2[:, 0:nt]), reads=[("xt", k), "rs2"], writes=[tk])
                    S.op("act", lambda e, k=k, nt=nt, tb=tb, hb=hb, c=c: e.activation(out=hb[:, k, 0:nt], in_=tb[:, 0:nt], func=AF.Identity,
                         scale=gs[c][:, k:k + 1], bias=v[:, k, 5 + 2 * c:6 + 2 * c]), reads=[tk, f"gs{c}", "v"], writes=[(hk, k)])
                for cc in range(2 * FC):
                    ps, pk = PS.get()
                    for k in range(KD):
                        S.op("pe", lambda e, cc=cc, k=k, ps=ps, hb=hb, nt=nt: e.matmul(ps[:, 0:nt], w16[:, k, cc * 128:(cc + 1) * 128], hb[:, k, 0:nt],
                             start=(k == 0), stop=(k == KD - 1)), reads=[("w16", k), (hk, k)], writes=[pk])
                    u_ = ub[nu % 3]; uk = f"ub{nu % 3}"; nu += 1
                    S.op("act" if nu % 2 else "dve", (lambda e, u_=u_, ps=ps, nt=nt: e.activation(out=u_[:, 0:nt], in_=ps[:, 0:nt], func=AF.Copy)) if nu % 2 else
                         (lambda e, u_=u_, ps=ps, nt=nt: e.tensor_copy(u_[:, 0:nt], ps[:, 0:nt])), reads=[pk], writes=[uk])
                    S.op("dma", lambda e, u_=u_, cc=cc, t0=t0, nt=nt: e.dma_start(out=U[cc, :, t0:t0 + nt], in_=u_[:, 0:nt]), reads=[uk], writes=[("U", cc, t0)])
            S.barrier()
        with ExitStack() as sst:
            sb = lambda n, s_, d: sst.enter_context(nc.sbuf_tensor("c_" + n, s_, d))
            cwt = sb("cw", [128, 2 * FC, 4], F32)
            S.op("dma", lambda e: e.dma_start(out=cwt[:], in_=cw), writes=["cw"])
            NT = 512
            ua = [sb(f"ua{i}", [128, NT + 2], F32) for i in range(2)]; ug = [sb(f"ug{i}", [128, NT + 2], F32) for i in range(2)]
            va = sb("va", [128, NT], F32); vg = sb("vg", [128, NT], F32); sg = sb("sg", [128, NT], F32); ob = [sb(f"ob{i}", [128, NT], BF16) for i in range(2)]
            it = 0
            for fc in range(FC):
                for (a0, n) in ((0, C), (C, N)):
                    for t0 in range(a0, a0 + n, NT):
                        nt = min(NT, a0 + n - t0); j = it % 2; it += 1
                        lo = max(a0, t0 - 1); hi = min(a0 + n, t0 + nt + 1)
                        for (buf, bk, cc) in ((ua[j], f"ua{j}", fc), (ug[j], f"ug{j}", FC + fc)):
                            S.op("dve", lambda e, buf=buf: e.memset(buf[:], 0.0), writes=[bk])
                            S.op("dma", lambda e, buf=buf, cc=cc, lo=lo, hi=hi, t0=t0: e.dma_start(out=buf[:, lo - (t0 - 1):hi - (t0 - 1)], in_=U[cc, :, lo:hi]),
                                 reads=[bk], writes=[bk])
                        for (buf, bk, cc, vout, vk) in ((ua[j], f"ua{j}", fc, va, "va"), (ug[j], f"ug{j}", FC + fc, vg, "vg")):
                            S.op("dve", lambda e, buf=buf, cc=cc, vout=vout, nt=nt: e.tensor_scalar(vout[:, 0:nt], buf[:, 1:nt + 1], cwt[:, cc, 1:2], cwt[:, cc, 3:4], ALU.mult, ALU.add), reads=[bk, "cw"], writes=[vk])
                            S.op("dve", lambda e, buf=buf, cc=cc, vout=vout, nt=nt: e.scalar_tensor_tensor(vout[:, 0:nt], buf[:, 0:nt], cwt[:, cc, 0:1], vout[:, 0:nt], ALU.mult, ALU.add), reads=[bk, "cw", vk], writes=[vk])
                            S.op("dve", lambda e, buf=buf, cc=cc, vout=vout, nt=nt: e.scalar_tensor_tensor(vout[:, 0:nt], buf[:, 2:nt + 2], cwt[:, cc, 2:3], vout[:, 0:nt], ALU.mult, ALU.add), reads=[bk, "cw", vk], writes=[vk])
                        S.op("act", lambda e, nt=nt: e.activation(out=sg[:, 0:nt], in_=vg[:, 0:nt], func=AF.Sigmoid), reads=["vg"], writes=["sg"])
                        S.op("dve", lambda e, nt=nt: e.tensor_mul(vg[:, 0:nt], vg[:, 0:nt], sg[:, 0:nt]), reads=["vg", "sg"], writes=["vg"])
                        o_ = ob[j]; ok_ = f"ob{j}"
                        S.op("dve", lambda e, o_=o_, nt=nt: e.tensor_mul(o_[:, 0:nt], va[:, 0:nt], vg[:, 0:nt]), reads=["va", "vg"], writes=[ok_])
                        S.op("dma", lambda e, o_=o_, fc=fc, t0=t0, nt=nt: e.dma_start(out=act[fc, :, t0:t0 + nt], in_=o_[:, 0:nt]), reads=[ok_], writes=[("act", fc, t0)])
            S.barrier()
        S.finish([])
    return nc


def build_p7(cfg):
    D = cfg["D"]; KD = D // 128; C, N = cfg["CTX"], cfg["SEQ"]; T = C + N; DS = D // 4; DC = DS // 128
    nc = bass.Bass("TRN2", target_bir_lowering=False)
    I = lambda n, shp, dt=F32: nc.dram_tensor(n, shp, dt, kind="ExternalInput").ap()
    zT = I("zT", [128, KD, T]); zo = I("zo", [128, DC, T]); xo = I("xo", [128, DC, T]); veco = I("veco", [128, DC, 4])
    out = nc.dram_tensor("x2T", [DC, 128, T], F32, kind="ExternalOutput").ap()
    with ExitStack() as st:
        S = Sched(nc, st); PS = PsumPool(nc, st, 8)
        sb = lambda n, s_, d: st.enter_context(nc.sbuf_tensor(n, s_, d))
        ones = sb("ones", [128, 128], BF16)
        S.op("dve", lambda e: e.memset(ones[:], 1.0), writes=["ones"])
        vo = sb("vo", [128, DC, 4], F32)
        S.op("dma", lambda e: e.dma_start(out=vo[:], in_=veco), writes=["vo"])
        ggo = [sb(f"ggo{i}", [128, DC], F32) for i in range(2)]
        for i in range(2):
            S.op("dve", lambda e, i=i: e.tensor_mul(ggo[i][:], vo[:, :, 0], vo[:, :, 1 + i]), reads=["vo"], writes=[f"ggo{i}"])
        NT = 512
        zt = sb("zt", [128, KD, NT], F32); sq = sb("sq", [128, KD, NT], BF16); rstd = sb("rstd", [128, NT], F32)
        zot = sb("zot", [128, DC, NT], F32); xot = sb("xot", [128, DC, NT], F32)
        fin = []
        for (t0, nt, is_ctx) in token_tiles(cfg):
            c = 1 if is_ctx else 0
            dma_groups(S, KD, 4, lambda e, k0, k1, t0=t0, nt=nt: e.dma_start(out=zt[:, k0:k1, 0:nt], in_=zT[:, k0:k1, t0:t0 + nt]), writes=["zt"])
            S.op("dma", lambda e, t0=t0, nt=nt: e.dma_start(out=zot[:, :, 0:nt], in_=zo[:, :, t0:t0 + nt]), writes=[("zot", k) for k in range(DC)])
            S.op("dma", lambda e, t0=t0, nt=nt: e.dma_start(out=xot[:, :, 0:nt], in_=xo[:, :, t0:t0 + nt]), writes=[("xot", k) for k in range(DC)])
            emit_rstd(nc, S, PS, ones, zt, KD, nt, D, rstd, sq, "zt")
            for k in range(DC):
                S.op("dve", lambda e, k=k, nt=nt: e.tensor_mul(zot[:, k, 0:nt], zot[:, k, 0:nt], rstd[:, 0:nt]), reads=[("zot", k), "rstd"], writes=[("zot", k)])
                S.op("dve", lambda e, k=k, nt=nt, c=c: e.scalar_tensor_tensor(xot[:, k, 0:nt], zot[:, k, 0:nt], ggo[c][:, k:k + 1], xot[:, k, 0:nt], ALU.mult, ALU.add),
                     reads=[("zot", k), f"ggo{c}", ("xot", k)], writes=[("xot", k)])
                S.op("dma", lambda e, k=k, t0=t0, nt=nt: e.dma_start(out=out[k, :, t0:t0 + nt], in_=xot[:, k, 0:nt]), reads=[("xot", k)], writes=[("out", k, t0)])
                fin.append(("out", k, t0))
        S.finish(fin)
    return nc


def unchunk(a):
    return a.reshape(-1, a.shape[-1])


def run_module(cfg, p, launch_fn=None):
    import ml_dtypes
    bf = ml_dtypes.bfloat16
    LA = launch_fn or launch
    D, L, B, C, N, F = cfg["D"], cfg["L"], cfg["B"], cfg["CTX"], cfg["SEQ"], cfg["F"]
    T = C + N; DS = D // 4; DC = DS // 128; FS = F // 4; KD = D // 128
    cores = [(c // 4, c % 4) for c in range(NCORES)]
    mod = run_p0(cfg, p["c"], p["c_ctx"], p["w_ada"], p["b_ada"]) if launch_fn is None else run_p0_with(cfg, p, LA)
    xfull = [np.ascontiguousarray(np.concatenate([p["ctx"][b], p["x"][b]], axis=0).T) for b in range(B)]
    progs = {}
    def prog(key, fn):
        if key not in progs:
            progs[key] = fn()
        return progs[key]
    mvec = lambda l, b, j: mod[l, b, j * D:(j + 1) * D]
    for l in range(L):
        ctx_out = l < L - 1
        nc1 = prog(("p1", ctx_out), lambda: build_p1(cfg, mixers=("wg", "na", "s5", "ml"), with_ctx_out=ctx_out))
        r1 = LA(nc1, [p1_inputs(cfg, l, b, h, xfull[b], mod, p["g_mix_pre"], p["w_in"], p["wg_sink"], p["na_rpb"], p) for (b, h) in cores])
        brF = [np.stack([np.asarray(r1[4 * b + h]["brT"]).reshape(4, 128, T) for h in range(4)], axis=0) for b in range(B)]
        brF = [np.ascontiguousarray(x.transpose(2, 1, 0, 3).reshape(128, 16, T)) for x in brF]
        hxT = [np.asarray(r1[4 * b]["hxT"]).reshape(128, KD, T) for b in range(B)]
        nc2 = prog("p2", lambda: build_p2(cfg))
        maps = []
        for (b, h) in cores:
            gcols = np.concatenate([p["w_in"][l][:, OFF["gate"] + i * D + h * DS: OFF["gate"] + i * D + (h + 1) * DS] for i in range(4)], axis=1)
            wb = np.stack([chunked(p["w_branch"][l][i][:, h * DS:(h + 1) * DS]) for i in range(4)], axis=1).reshape(128, 16, DS)
            maps.append({"hxT": hxT[b], "wg": chunked(gcols), "brF": brF[b], "wb": np.ascontiguousarray(wb),
                         "wglu": chunked(p["s5_w_glu"][l]), "bglu": np.ascontiguousarray(p["s5_b_glu"][l].reshape(4, 128).T)})
        r2 = LA(nc2, maps)
        merged = [np.concatenate([unchunk(np.asarray(r2[4 * b + h]["mergedT"]).reshape(DC, 128, T)) for h in range(4)], axis=0) for b in range(B)]
        nc3 = prog(("mat", D), lambda: build_matT(cfg, D))
        r3 = LA(nc3, [{"aT": chunked(merged[b]), "w": chunked(p["w_out"][l][:, h * DS:(h + 1) * DS])} for (b, h) in cores])
        y = [np.concatenate([unchunk(np.asarray(r3[4 * b + h]["yT"]).reshape(DC, 128, T)) for h in range(4)], axis=0) for b in range(B)]
        nc5 = prog("p5", lambda: build_p5(cfg))
        maps = []
        for (b, h) in cores:
            sl = slice(h * DS, (h + 1) * DS)
            vec = np.stack([p["g_mix_post"][l], mvec(l, b, 2), mvec(l, 2, 2), p["g_ffn_pre"][l], mvec(l, b, 4), mvec(l, b, 3), mvec(l, 2, 4), mvec(l, 2, 3)], axis=1)
            veco = np.stack([p["g_mix_post"][l][sl], mvec(l, b, 2)[sl], mvec(l, 2, 2)[sl], np.zeros(DS, np.float32)], axis=1)
            cols = np.concatenate([np.arange(h * FS, (h + 1) * FS), F + np.arange(h * FS, (h + 1) * FS)])
            cwv = np.concatenate([p["ffn_conv_w"][l][:, cols], p["ffn_conv_b"][l][None, cols]], axis=0).T
            maps.append({"xT": chunked(xfull[b]), "yT": chunked(y[b]), "xo": chunked(xfull[b][sl]), "yo": chunked(y[b][sl]),
                         "vec": chunked(vec.astype(np.float32)), "veco": chunked(veco.astype(np.float32)),
                         "wup": chunked(p["w_up"][l][:, cols]), "cw": chunked(np.ascontiguousarray(cwv.astype(np.float32)))})
        r5 = LA(nc5, maps)
        x1 = [np.concatenate([unchunk(np.asarray(r5[4 * b + h]["x1T"]).reshape(DC, 128, T)) for h in range(4)], axis=0) for b in range(B)]
        act = [np.concatenate([unchunk(np.asarray(r5[4 * b + h]["actT"]).reshape(FS // 128, 128, T)) for h in range(4)], axis=0) for b in range(B)]
        nc6 = prog(("mat", F), lambda: build_matT(cfg, F))
        r6 = LA(nc6, [{"aT": chunked(act[b]), "w": chunked(p["w_down"][l][:, h * DS:(h + 1) * DS])} for (b, h) in cores])
        z = [np.concatenate([unchunk(np.asarray(r6[4 * b + h]["yT"]).reshape(DC, 128, T)) for h in range(4)], axis=0) for b in range(B)]
        nc7 = prog("p7", lambda: build_p7(cfg))
        maps = []
        for (b, h) in cores:
            sl = slice(h * DS, (h + 1) * DS)
            veco = np.stack([p["g_ffn_post"][l][sl], mvec(l, b, 5)[sl], mvec(l, 2, 5)[sl], np.zeros(DS, np.float32)], axis=1)
            maps.append({"zT": chunked(z[b]), "zo": chunked(z[b][sl]), "xo": chunked(x1[b][sl]), "veco": chunked(veco.astype(np.float32))})
        r7 = LA(nc7, maps)
        xfull = [np.concatenate([unchunk(np.asarray(r7[4 * b + h]["x2T"]).reshape(DC, 128, T)) for h in range(4)], axis=0) for b in range(B)]
    return np.ascontiguousarray(np.stack([xfull[b][:, C:].T for b in range(B)], axis=0)).astype(np.float32)


def run_p0_with(cfg, p, LA):
    global launch
    old = launch
    try:
        launch = LA
        return run_p0(cfg, p["c"], p["c_ctx"], p["w_ada"], p["b_ada"])
    finally:
        launch = old


def kernel(**inputs):
    p = {k: np.asarray(v) for k, v in inputs.items()}
    return run_module(FULL, p)
```
